# Optimizing a Trainium2 kernel written in Bass

```python
import jax, jax.numpy as jnp
from jax import lax
import numpy as np

D_MODEL = 1024
BATCH = 8
SEQ = 4096
DEPTH = 2

GRID_W = 64
CTX_LEN = 256
HG_WIDTH = D_MODEL // 2
HG_HEADS = HG_WIDTH // 128
HG_DK = HG_WIDTH // HG_HEADS
HG_DV = HG_DK
ML_WIDTH = D_MODEL - HG_WIDTH
ML_HEADS = 4
ML_DK = ML_WIDTH // ML_HEADS // 2
ML_DV = ML_WIDTH // ML_HEADS
ML_CONV = 3
MIX_WIDTH = HG_WIDTH + ML_WIDTH
PROJ_DIM = 5 * HG_WIDTH + 2 * ML_HEADS * ML_DK + 2 * ML_WIDTH + 4 * ML_HEADS
CHUNK = 64
D_FF = 2816
N_EXPERTS = 8
TOP_K = 2
D_EXPERT = D_FF // TOP_K
DEEPNORM_ALPHA = (2 * DEPTH) ** 0.25
DEEPNORM_BETA = (8 * DEPTH) ** -0.25
LN_EPS = 1e-5
RMS_EPS = 1e-6

kernel_name = "hgrn2_mlstm_hybrid_dit_moe"


def _layer_norm(x, g, b):
    xf = x.astype(jnp.float32)
    mu = jnp.mean(xf, axis=-1, keepdims=True)
    var = jnp.mean(jnp.square(xf - mu), axis=-1, keepdims=True)
    y = (xf - mu) * lax.rsqrt(var + LN_EPS) * g.astype(jnp.float32) + b.astype(jnp.float32)
    return y.astype(x.dtype)


def _head_rms(x, g):
    return x * lax.rsqrt(jnp.mean(x * x, axis=-1, keepdims=True) + RMS_EPS) * g.astype(jnp.float32)


def _centred_dwconv(u, w, b):
    width = w.shape[0]
    half = width // 2
    n = u.shape[-2]
    up = jnp.pad(u, [(0, 0)] * (u.ndim - 2) + [(half, half), (0, 0)])
    out = b
    for t in range(width):
        out = out + up[..., t:t + n, :] * w[t]
    return out


def _to_chunks(t):
    bsz, n, h = t.shape[:3]
    t = t.reshape(bsz, n // CHUNK, CHUNK, h, *t.shape[3:])
    return jnp.moveaxis(t, (1, 3), (0, 2))


def _from_chunks(t):
    n, bsz, h, c = t.shape[:4]
    return jnp.moveaxis(t, (0, 2), (1, 3)).reshape(bsz, n * c, h, *t.shape[4:])


def _hgrn2_scan(q, v, log_f, k, s0):
    causal = jnp.tril(jnp.ones((CHUNK, CHUNK), dtype=bool))[:, :, None]

    def step(s, xs):
        qc, vc, fc, kc = xs
        g = jnp.cumsum(fc, axis=2)
        decay = jnp.exp(jnp.where(causal, g[:, :, :, None, :] - g[:, :, None, :, :], -jnp.inf))
        a = jnp.einsum("bhid,bhjd,bhijd->bhij", qc, kc, decay)
        o = jnp.einsum("bhij,bhjv->bhiv", a, vc) + jnp.einsum("bhid,bhdv->bhiv", qc * jnp.exp(g), s)
        g_end = g[:, :, -1, :]
        s_new = jnp.exp(g_end)[..., None] * s + jnp.einsum(
            "bhjd,bhjv->bhdv", kc * jnp.exp(g_end[:, :, None, :] - g), vc)
        return s_new, o

    xs = (_to_chunks(q), _to_chunks(v), _to_chunks(log_f), _to_chunks(k))
    s_fin, o = lax.scan(step, s0, xs)
    return _from_chunks(o), s_fin


def _mlstm_scan(q, k, v, log_i, log_f, state):
    causal = jnp.tril(jnp.ones((CHUNK, CHUNK), dtype=bool))

    def step(carry, xs):
        c_prev, n_prev, m_prev = carry
        qc, kc, vc, ic, fc = xs
        b = jnp.cumsum(fc, axis=-1)
        d = jnp.where(causal, b[..., :, None] - b[..., None, :] + ic[..., None, :], -jnp.inf)
        inter = b + m_prev[..., None]
        m = jnp.maximum(jnp.max(d, axis=-1), inter)
        s = jnp.einsum("bhid,bhjd->bhij", qc, kc) * jnp.exp(d - m[..., None])
        w_inter = jnp.exp(inter - m)[..., None]
        num = jnp.einsum("bhij,bhjv->bhiv", s, vc) + w_inter * jnp.einsum("bhid,bhdv->bhiv", qc, c_prev)
        den = jnp.sum(s, axis=-1) + w_inter[..., 0] * jnp.einsum("bhid,bhd->bhi", qc, n_prev)
        h = num / jnp.maximum(jnp.abs(den), jnp.exp(-m))[..., None]
        m_new = m[..., -1]
        w_k = jnp.exp(b[..., -1:] - b + ic - m_new[..., None])[..., None] * kc
        dec = jnp.exp(b[..., -1] + m_prev - m_new)
        c_new = dec[..., None, None] * c_prev + jnp.einsum("bhjd,bhjv->bhdv", w_k, vc)
        n_new = dec[..., None] * n_prev + jnp.sum(w_k, axis=2)
        return (c_new, n_new, m_new), h

    xs = (_to_chunks(q), _to_chunks(k), _to_chunks(v), _to_chunks(log_i), _to_chunks(log_f))
    state, h = lax.scan(step, state, xs)
    return _from_chunks(h), state


def _bidir(scan_fn, shared, dirs, init):
    out_f, st_f = scan_fn(*shared, *dirs[0], init[0])
    flip = lambda t: jnp.flip(t, axis=1)
    out_b, st_b = scan_fn(*[flip(t) for t in shared], *[flip(t) for t in dirs[1]], init[1])
    return out_f + flip(out_b), (st_f, st_b)


def _stream_features(z, conv_w, conv_b, lb, gate_b, rows):
    bsz, n, _ = z.shape
    f32 = jnp.float32
    sizes = (HG_WIDTH,) * 5 + (2 * ML_HEADS * ML_DK, ML_WIDTH, 4 * ML_HEADS, ML_WIDTH)
    cuts, acc = [], 0
    for s in sizes[:-1]:
        acc += s
        cuts.append(acc)
    hq, hf_fwd, hf_bwd, hi, hg, mqk, mv, mgates, mo = jnp.split(z, cuts, axis=-1)
    hg_dirs = []
    for zf, lbd in ((hf_fwd, lb[0]), (hf_bwd, lb[1])):
        zf = zf.astype(f32)
        lbd = lbd.astype(f32)
        log_f = jnp.logaddexp(jnp.log(lbd), jnp.log1p(-lbd) + jax.nn.log_sigmoid(zf))
        k = (1.0 - lbd) * jax.nn.sigmoid(-zf)
        hg_dirs.append((log_f.reshape(bsz, n, HG_HEADS, HG_DK), k.reshape(bsz, n, HG_HEADS, HG_DK)))
    if rows is None:
        qk = _centred_dwconv(mqk, conv_w, conv_b)
    else:
        qk = _centred_dwconv(mqk.reshape(bsz, rows, GRID_W, -1), conv_w, conv_b).reshape(bsz, n, -1)
    mq, mk = jnp.split(jax.nn.silu(qk), 2, axis=-1)
    gates = (mgates.astype(f32) + gate_b.reshape(-1).astype(f32)).reshape(bsz, n, 2, 2, ML_HEADS)
    ml_dirs = [(gates[:, :, d, 0], jax.nn.log_sigmoid(gates[:, :, d, 1])) for d in range(2)]
    return {
        "hg_q": jax.nn.silu(hq).astype(f32).reshape(bsz, n, HG_HEADS, HG_DK),
        "hg_v": hi.astype(f32).reshape(bsz, n, HG_HEADS, HG_DV),
        "hg_dirs": hg_dirs,
        "hg_gate": hg,
        "ml_q": mq.astype(f32).reshape(bsz, n, ML_HEADS, ML_DK) * (ML_DK ** -0.5),
        "ml_k": mk.astype(f32).reshape(bsz, n, ML_HEADS, ML_DK),
        "ml_v": mv.astype(f32).reshape(bsz, n, ML_HEADS, ML_DV),
        "ml_dirs": ml_dirs,
        "ml_gate": mo,
    }


def _mix_out(hg_o, ml_h, feats, hg_norm, ml_norm, w_out, dtype):
    bsz, n = hg_o.shape[:2]
    hg = _head_rms(hg_o, hg_norm) * jax.nn.silu(feats["hg_gate"].astype(jnp.float32)).reshape(bsz, n, HG_HEADS, HG_DV)
    ml = _head_rms(ml_h, ml_norm) * jax.nn.sigmoid(feats["ml_gate"].astype(jnp.float32)).reshape(bsz, n, ML_HEADS, ML_DV)
    y = jnp.concatenate([hg.reshape(bsz, n, HG_WIDTH), ml.reshape(bsz, n, ML_WIDTH)], axis=-1)
    return y.astype(dtype) @ w_out


def _token_mix(hx, hc, rows, w_in, conv_w, conv_b, lb, gate_b, hg_norm, ml_norm, w_out, with_ctx_out):
    bsz = hx.shape[0]
    f32 = jnp.float32
    fx = _stream_features(hx @ w_in, conv_w, conv_b, lb, gate_b, rows)
    fc = _stream_features(hc @ w_in, conv_w, conv_b, lb, gate_b, None)
    hg0 = jnp.zeros((bsz, HG_HEADS, HG_DK, HG_DV), f32)
    ml0 = (jnp.zeros((bsz, ML_HEADS, ML_DK, ML_DV), f32), jnp.zeros((bsz, ML_HEADS, ML_DK), f32),
           jnp.zeros((bsz, ML_HEADS), f32))
    hg_c, hg_state = _bidir(_hgrn2_scan, (fc["hg_q"], fc["hg_v"]), fc["hg_dirs"], (hg0, hg0))
    ml_c, ml_state = _bidir(_mlstm_scan, (fc["ml_q"], fc["ml_k"], fc["ml_v"]), fc["ml_dirs"], (ml0, ml0))
    hg_x, _ = _bidir(_hgrn2_scan, (fx["hg_q"], fx["hg_v"]), fx["hg_dirs"], hg_state)
    ml_x, _ = _bidir(_mlstm_scan, (fx["ml_q"], fx["ml_k"], fx["ml_v"]), fx["ml_dirs"], ml_state)
    out_x = _mix_out(hg_x, ml_x, fx, hg_norm, ml_norm, w_out, hx.dtype)
    out_c = _mix_out(hg_c, ml_c, fc, hg_norm, ml_norm, w_out, hc.dtype) if with_ctx_out else None
    return out_x, out_c


def _swiglu(h, w_gate_up, w_down):
    gate, up = jnp.split(h @ w_gate_up, 2, axis=-1)
    return (jax.nn.silu(gate) * up) @ w_down


def _moe_swiglu(h, router_w, router_b, w_gate_up, w_down):
    logits = (h @ router_w).astype(jnp.float32) + router_b.astype(jnp.float32)
    top_val, top_idx = lax.top_k(logits, TOP_K)
    top_p = jax.nn.softmax(top_val, axis=-1)
    combine = jnp.einsum("blk,blke->ble", top_p, jax.nn.one_hot(top_idx, N_EXPERTS, dtype=jnp.float32)).astype(h.dtype)
    out = jnp.zeros_like(h)
    for e in range(N_EXPERTS):
        out = out + combine[..., e:e + 1] * _swiglu(h, w_gate_up[e], w_down[e])
    return out


def _channel_mix(h, layer, ffn_w_gate_up, ffn_w_down, router_w, router_b, moe_w_gate_up, moe_w_down):
    j = layer // 2
    if layer % 2 == 0:
        return _swiglu(h, ffn_w_gate_up[j], ffn_w_down[j])
    return _moe_swiglu(h, router_w[j], router_b[j], moe_w_gate_up[j], moe_w_down[j])


def setup_inputs(seed: int = 0) -> dict:
    key = jax.random.key(seed)
    ks = jax.random.split(key, 24)
    nrm = jax.random.normal
    f32 = jnp.float32
    n_dense = (DEPTH + 1) // 2
    n_moe = DEPTH // 2
    d_in = D_MODEL ** -0.5
    ig_b = 0.1 * nrm(ks[10], (DEPTH, 2, 1, ML_HEADS), f32)
    fg_b = jnp.linspace(3.0, 6.0, ML_HEADS, dtype=f32) + 0.1 * nrm(ks[11], (DEPTH, 2, 1, ML_HEADS), f32)
    return {
        "x": nrm(ks[0], (BATCH, SEQ, D_MODEL), f32),
        "c": nrm(ks[1], (BATCH, D_MODEL), f32),
        "ctx": nrm(ks[2], (BATCH, CTX_LEN, D_MODEL), f32),
        "c_ctx": nrm(ks[3], (D_MODEL,), f32),
        "w_ada": 0.5 * d_in * nrm(ks[4], (DEPTH, D_MODEL, 6 * D_MODEL), f32),
        "b_ada": 0.02 * nrm(ks[5], (DEPTH, 6 * D_MODEL), f32),
        "w_in": d_in * nrm(ks[6], (DEPTH, D_MODEL, PROJ_DIM), f32),
        "ml_conv_w": (ML_CONV ** -0.5) * nrm(ks[7], (DEPTH, ML_CONV, 2 * ML_HEADS * ML_DK), f32),
        "ml_conv_b": 0.02 * nrm(ks[8], (DEPTH, 2 * ML_HEADS * ML_DK), f32),
        "hg_lower_bound": 0.1 * nrm(ks[9], (DEPTH, 2, HG_WIDTH), f32),
        "ml_gate_bias": jnp.concatenate([ig_b, fg_b], axis=2),
        "hg_norm": 1.0 + 0.05 * nrm(ks[12], (DEPTH, HG_HEADS, HG_DV), f32),
        "ml_norm": 1.0 + 0.05 * nrm(ks[13], (DEPTH, ML_HEADS, ML_DV), f32),
        "w_out": DEEPNORM_BETA * (MIX_WIDTH ** -0.5) * nrm(ks[14], (DEPTH, MIX_WIDTH, D_MODEL), f32),
        "ln_g": 1.0 + 0.05 * nrm(ks[15], (DEPTH, 2, D_MODEL), f32),
        "ln_b": 0.02 * nrm(ks[16], (DEPTH, 2, D_MODEL), f32),
        "ffn_w_gate_up": d_in * nrm(ks[17], (n_dense, D_MODEL, 2 * D_FF), f32),
        "ffn_w_down": DEEPNORM_BETA * (D_FF ** -0.5) * nrm(ks[18], (n_dense, D_FF, D_MODEL), f32),
        "router_w": d_in * nrm(ks[19], (n_moe, D_MODEL, N_EXPERTS), f32),
        "router_b": 0.01 * nrm(ks[20], (n_moe, N_EXPERTS), f32),
        "moe_w_gate_up": d_in * nrm(ks[21], (n_moe, N_EXPERTS, D_MODEL, 2 * D_EXPERT), f32),
        "moe_w_down": DEEPNORM_BETA * (D_EXPERT ** -0.5) * nrm(ks[22], (n_moe, N_EXPERTS, D_EXPERT, D_MODEL), f32),
    }


def reference(x, c, ctx, c_ctx, w_ada, b_ada, w_in, ml_conv_w, ml_conv_b, hg_lower_bound, ml_gate_bias,
              hg_norm, ml_norm, w_out, ln_g, ln_b, ffn_w_gate_up, ffn_w_down, router_w, router_b,
              moe_w_gate_up, moe_w_down):
    bsz, seq, d = x.shape
    rows = seq // GRID_W
    lb_all = jnp.cumsum(jax.nn.softmax(hg_lower_bound.astype(jnp.float32), axis=0), axis=0)
    lb_all = lb_all - lb_all[0]
    cond_x = jax.nn.silu(c)
    cond_c = jax.nn.silu(c_ctx)
    for l in range(DEPTH):
        last = l == DEPTH - 1
        mod_x = (cond_x @ w_ada[l] + b_ada[l]).reshape(bsz, 6, 1, d)
        mod_c = (cond_c @ w_ada[l] + b_ada[l]).reshape(6, 1, d)
        hx = x * (1.0 + mod_x[:, 1]) + mod_x[:, 0]
        hc = ctx * (1.0 + mod_c[1]) + mod_c[0]
        mix_x, mix_c = _token_mix(hx, hc, rows, w_in[l], ml_conv_w[l], ml_conv_b[l], lb_all[l], ml_gate_bias[l],
                                  hg_norm[l], ml_norm[l], w_out[l], not last)
        x = _layer_norm(DEEPNORM_ALPHA * x + mod_x[:, 2] * mix_x, ln_g[l, 0], ln_b[l, 0])
        if not last:
            ctx = _layer_norm(DEEPNORM_ALPHA * ctx + mod_c[2] * mix_c, ln_g[l, 0], ln_b[l, 0])
        hx = x * (1.0 + mod_x[:, 4]) + mod_x[:, 3]
        f_x = _channel_mix(hx, l, ffn_w_gate_up, ffn_w_down, router_w, router_b, moe_w_gate_up, moe_w_down)
        x = _layer_norm(DEEPNORM_ALPHA * x + mod_x[:, 5] * f_x, ln_g[l, 1], ln_b[l, 1])
        if not last:
            hc = ctx * (1.0 + mod_c[4]) + mod_c[3]
            f_c = _channel_mix(hc, l, ffn_w_gate_up, ffn_w_down, router_w, router_b, moe_w_gate_up, moe_w_down)
            ctx = _layer_norm(DEEPNORM_ALPHA * ctx + mod_c[5] * f_c, ln_g[l, 1], ln_b[l, 1])
    return x
```

```python
import math
import os
import threading
from contextlib import ExitStack
import numpy as np
import concourse.bass as bass
import concourse.mybir as mybir
from concourse.bass_utils import run_bass_kernel_spmd

F32 = mybir.dt.float32
BF16 = mybir.dt.bfloat16
AF = mybir.ActivationFunctionType
ALU = mybir.AluOpType
AX = mybir.AxisListType

D = 1024
KC = 8
CTX = 256
PROJ = 4112
D_FF = 2816
NE = 8
D_EXP = 1408
DEPTH = 2
ALPHA = (2 * DEPTH) ** 0.25
LN_EPS = 1e-5
RMS_EPS = 1e-6


class T:
    __slots__ = ("name", "ap", "w", "rs", "dsem", "dcnt", "dload", "dwaited", "dbase")

    def __init__(self, name, ap):
        self.name = name
        self.ap = ap
        self.w = None
        self.rs = {}
        self.dsem = None
        self.dcnt = 0
        self.dload = 0
        self.dwaited = {}
        self.dbase = 0

    def __getitem__(self, k):
        return self.ap[k]


class E:
    def __init__(self, name, eng, sem):
        self.name = name
        self.e = eng
        self.sem = sem
        self.count = 0
        self.waited = {}


class Ctx:
    NO_SELF_SYNC = tuple(os.environ.get('MK_NOSELF', '').split(','))

    def __init__(self, nc, stack):
        self.nc = nc
        self.gstack = stack
        self.stack = stack
        self.engs = {}
        for name, eng in (("pe", nc.tensor), ("act", nc.scalar), ("dve", nc.vector),
                          ("pool", nc.gpsimd), ("sp", nc.sync)):
            sem = stack.enter_context(nc.semaphore("s_" + name))
            self.engs[name] = E(name, eng, sem)
        self.live = []
        self.uid = 0
        self.free_dsems = {"sw": [], "hw": []}
        self.lane_hook = None

    def sb(self, name, shape, dt):
        self.uid += 1
        t = self.stack.enter_context(self.nc.sbuf_tensor(f"{name}_{self.uid}", list(shape), dt))
        tt = T(name, t)
        self.live.append(tt)
        return tt

    def ps(self, name, shape, dt=F32):
        t = self.stack.enter_context(self.nc.psum_tensor(name, list(shape), dt))
        tt = T(name, t)
        self.live.append(tt)
        return tt

    def _dsem(self, t, q):
        kind = "sw" if q == "pool" else "hw"
        if t.dsem is None:
            if self.free_dsems[kind]:
                t.dsem = self.free_dsems[kind].pop()
            else:
                self.uid += 1
                t.dsem = [self.gstack.enter_context(self.nc.semaphore(f"d{self.uid}")), 0, kind]
            t.dbase = t.dsem[1]
        assert t.dsem[2] == kind, (t.name, kind)
        return t.dsem[0]

    def _need(self, en, rd, wr):
        Eo = self.engs[en]
        deps = {}

        def add(dep):
            if dep is None:
                return
            e2, c2 = dep
            if e2 == en and (en == "pe" or en in self.NO_SELF_SYNC):
                return
            if deps.get(e2, 0) < c2:
                deps[e2] = c2

        dwaits = []
        for t in rd:
            add(t.w)
            if t.dload > t.dwaited.get(en, 0):
                dwaits.append((t, t.dload))
        for t in wr:
            add(t.w)
            for e2, c2 in t.rs.items():
                add((e2, c2))
            if t.dcnt > t.dwaited.get(en, 0):
                dwaits.append((t, t.dcnt))
        for e2, c2 in deps.items():
            if Eo.waited.get(e2, 0) < c2:
                Eo.e.wait_ge(self.engs[e2].sem, c2)
                Eo.waited[e2] = c2
        for t, c in dwaits:
            if t.dwaited.get(en, 0) < c:
                Eo.e.wait_ge(t.dsem[0], t.dbase + 16 * c)
                t.dwaited[en] = c

    def op(self, en, fn, rd=(), wr=()):
        Eo = self.engs[en]
        self._need(en, rd, wr)
        ins = fn(Eo.e)
        Eo.count += 1
        ins.then_inc(Eo.sem, 1)
        c = Eo.count
        for t in rd:
            if t.rs.get(en, 0) < c:
                t.rs[en] = c
        for t in wr:
            t.w = (en, c)
            t.rs = {}
        if self.lane_hook is not None:
            self.lane_hook()
        return ins

    def dma(self, q, out, in_, rd=None, wr=None):
        Eo = self.engs[q]
        rdl = [rd] if rd is not None else []
        wrl = [wr] if wr is not None else []
        self._need(q, rdl, wrl)
        ins = Eo.e.dma_start(out=out, in_=in_)
        t = wr if wr is not None else rd
        sem = self._dsem(t, q)
        t.dcnt += 1
        ins.then_inc(sem, 16)
        if wr is not None:
            wr.dload = wr.dcnt
            wr.w = None
            wr.rs = {}
        return ins

    def barrier(self):
        names = list(self.engs)
        for en in names:
            Eo = self.engs[en]
            for e2 in names:
                if e2 == en:
                    continue
                c2 = self.engs[e2].count
                if c2 > Eo.waited.get(e2, 0):
                    Eo.e.wait_ge(self.engs[e2].sem, c2)
                    Eo.waited[e2] = c2
            for t in self.live:
                if t.dcnt > t.dwaited.get(en, 0):
                    Eo.e.wait_ge(t.dsem[0], t.dbase + 16 * t.dcnt)
                    t.dwaited[en] = t.dcnt

    def phase(self):
        return _Phase(self)


class _Lanes:
    def __init__(self, n):
        self.alive = [True] * n
        self.cur = 0
        self.cv = threading.Condition()
        self.err = None

    def _advance(self):
        n = len(self.alive)
        for d in range(1, n + 1):
            j = (self.cur + d) % n
            if self.alive[j]:
                self.cur = j
                return
        self.cur = -1

    def switch(self):
        with self.cv:
            me = self.cur
            self._advance()
            if self.cur == me:
                return
            self.cv.notify_all()
            while self.cur != me:
                self.cv.wait()


def run_lanes(K, fns):
    if len(fns) == 0:
        return
    if len(fns) == 1:
        fns[0]()
        return
    L = _Lanes(len(fns))

    def worker(i, fn):
        with L.cv:
            while L.cur != i:
                L.cv.wait()
        try:
            fn()
        except BaseException as e:
            L.err = e
        with L.cv:
            L.alive[i] = False
            L._advance()
            L.cv.notify_all()

    K.lane_hook = L.switch
    ths = [threading.Thread(target=worker, args=(i, f)) for i, f in enumerate(fns)]
    for t in ths:
        t.start()
    for t in ths:
        t.join()
    K.lane_hook = None
    if L.err is not None:
        raise L.err


class _Phase:
    def __init__(self, K):
        self.K = K

    def __enter__(self):
        self.prev = self.K.stack
        self.mark = len(self.K.live)
        self.st = ExitStack()
        self.st.__enter__()
        self.K.stack = self.st
        return self

    def __exit__(self, *a):
        self.K.barrier()
        for t in self.K.live[self.mark:]:
            if t.dsem is not None:
                t.dsem[1] = t.dbase + 16 * t.dcnt
                self.K.free_dsems[t.dsem[2]].append(t.dsem)
                t.dsem = None
        del self.K.live[self.mark:]
        self.K.stack = self.prev
        self.st.__exit__(*a)
        return False


def mm(K, out, lhsT, rhs, rd, wr, start=True, stop=True, tp=None):
    if tp is None:
        return K.op("pe", lambda e: e.matmul(out, lhsT=lhsT, rhs=rhs, start=start, stop=stop), rd=rd, wr=wr)
    return K.op("pe", lambda e: e.matmul(out, lhsT=lhsT, rhs=rhs, start=start, stop=stop, tile_position=tp),
                rd=rd, wr=wr)


def act(K, out, in_, func, rd, wr, bias=None, scale=None):
    kw = {}
    if bias is not None:
        kw["bias"] = bias
    if scale is not None:
        kw["scale"] = scale
    return K.op("act", lambda e: e.activation(out=out, in_=in_, func=func, **kw), rd=rd, wr=wr)


def tt(K, en, out, in0, in1, op, rd, wr):
    return K.op(en, lambda e: e.tensor_tensor(out=out, in0=in0, in1=in1, op=op), rd=rd, wr=wr)


def ts(K, en, out, in0, s1, s2, op0, op1, rd, wr):
    if op1 is None:
        return K.op(en, lambda e: e.tensor_scalar(out=out, in0=in0, scalar1=s1, scalar2=None, op0=op0), rd=rd, wr=wr)
    return K.op(en, lambda e: e.tensor_scalar(out=out, in0=in0, scalar1=s1, scalar2=s2, op0=op0, op1=op1),
                rd=rd, wr=wr)


def stt(K, out, in0, scalar, in1, op0, op1, rd, wr):
    return K.op("dve", lambda e: e.scalar_tensor_tensor(out=out, in0=in0, scalar=scalar, in1=in1, op0=op0, op1=op1),
                rd=rd, wr=wr)


def cp(K, en, out, in_, rd, wr):
    if en == "act":
        return K.op("act", lambda e: e.copy(out=out, in_=in_), rd=rd, wr=wr)
    return K.op(en, lambda e: e.tensor_copy(out=out, in_=in_), rd=rd, wr=wr)


import os
STOP = int(os.environ.get("MK_STOP", "0"))


class _Stop(Exception):
    pass


def stop_at(n):
    if STOP == n:
        raise _Stop()


def build_program(SEQ):
    nc = bass.Bass("TRN2", target_bir_lowering=False)
    try:
        _build_body(nc, SEQ)
    except _Stop:
        pass
    return nc


def _build_body(nc, SEQ):
    NLT = SEQ // 128
    NCT = CTX // 128
    NG = NLT + NCT

    def din(name, shape):
        return nc.dram_tensor(name, list(shape), F32, kind="ExternalInput").ap()

    x_d = din("x", [SEQ, D])
    c_d = din("c", [1, D])
    ctx_d = din("ctx", [CTX, D])
    cctx_d = din("c_ctx", [1, D])
    w_ada = din("w_ada", [DEPTH, D, 6 * D])
    b_ada = din("b_ada", [DEPTH, 6 * D])
    w_in = din("w_in", [DEPTH, D, PROJ])
    conv_w = din("ml_conv_w", [DEPTH, 3, 512])
    conv_b = din("ml_conv_b", [DEPTH, 512])
    hlb = din("hg_lower_bound", [DEPTH, 2, 512])
    mgb = din("ml_gate_bias", [DEPTH, 16])
    hg_norm = din("hg_norm", [DEPTH, 512])
    ml_norm = din("ml_norm", [DEPTH, 512])
    w_out = din("w_out", [DEPTH, D, D])
    ln_g = din("ln_g", [DEPTH, 2, D])
    ln_b = din("ln_b", [DEPTH, 2, D])
    ffn_gu = din("ffn_w_gate_up", [1, D, 2 * D_FF])
    ffn_dn = din("ffn_w_down", [1, D_FF, D])
    router_w = din("router_w", [1, D, NE])
    router_b = din("router_b", [1, NE])
    moe_gu = din("moe_w_gate_up", [1, NE, D, 2 * D_EXP])
    moe_dn = din("moe_w_down", [1, NE, D_EXP, D])
    out_d = nc.dram_tensor("out", [SEQ, D], F32, kind="ExternalOutput").ap()

    X1 = nc.dram_tensor("X1", [NG * 128, D], F32, kind="Internal").ap()
    X2 = nc.dram_tensor("X2", [NG * 128, D], F32, kind="Internal").ap()
    OB = nc.dram_tensor("OB", [NG * 128, D], F32, kind="Internal").ap()

    with ExitStack() as gst:
        K = Ctx(nc, gst)
        identb = K.sb("identb", [128, 128], BF16)
        revb = K.sb("revb", [128, 128], BF16)
        identf = K.sb("identf", [128, 128], F32)
        revf = K.sb("revf", [128, 128], F32)
        trif = K.sb("trif", [128, 128], F32)
        onesf = K.sb("onesf", [128, 128], F32)
        trib = K.sb("trib", [128, 128], BF16)
        onesb = K.sb("onesb", [128, 128], BF16)
        mask4 = K.sb("mask4", [128, 4, 128], F32)
        rmask = K.sb("rmask", [128, 512], F32)
        hmask = K.sb("hmask", [128, 2], F32)
        smT = K.sb("smT", [128, 256], F32)
        condT = K.sb("condT", [128, 8, 2], BF16)
        condR = K.sb("condR", [128, 8, 2, 128], BF16)
        lbT = K.sb("lbT", [128, 2, 2, 4], F32)
        omlT = K.sb("omlT", [128, 2, 2, 4], F32)
        PB = [K.ps(f"bank{i}", [128, 512], F32) for i in range(8)]

        def mk_sel(t, pattern, base, cm, cmp, fill_in, fill):
            K.op("pool", lambda e: e.memset(t[:], fill_in), wr=[t])
            K.op("pool", lambda e: e.affine_select(out=t[:], in_=t[:], pattern=pattern, compare_op=cmp,
                                                   fill=fill, base=base, channel_multiplier=cm), rd=[t], wr=[t])

        mk_sel(identb, [[-1, 128]], 0, 1, ALU.not_equal, 0.0, 1.0)
        mk_sel(identf, [[-1, 128]], 0, 1, ALU.not_equal, 0.0, 1.0)
        mk_sel(revb, [[1, 128]], -127, 1, ALU.not_equal, 0.0, 1.0)
        mk_sel(revf, [[1, 128]], -127, 1, ALU.not_equal, 0.0, 1.0)
        mk_sel(trif, [[1, 128]], 0, -1, ALU.is_ge, 1.0, 0.0)
        mk_sel(mask4, [[0, 4], [1, 128]], 0, -1, ALU.is_ge, 1.0, 0.0)
        K.op("pool", lambda e: e.memset(onesf[:], 1.0), wr=[onesf])
        K.op("pool", lambda e: e.memset(onesb[:], 1.0), wr=[onesb])
        cp(K, "pool", trib[:], trif[:], rd=[trif], wr=[trib])
        K.op("pool", lambda e: e.memset(rmask[:], 1.0), wr=[rmask])
        rmv = rmask[:].rearrange("p (s j) -> p s j", j=128)
        K.op("pool", lambda e: e.memset(rmv[:, :, 0:1], 0.0), wr=[rmask])
        K.op("pool", lambda e: e.memset(hmask[:], 0.0), wr=[hmask])
        K.op("pool", lambda e: e.memset(hmask[0:64, 0:1], 1.0), wr=[hmask])
        K.op("pool", lambda e: e.memset(hmask[64:128, 1:2], 1.0), wr=[hmask])

        rows = []
        def add_rows(ap, n):
            r0 = sum(r for _, r in rows)
            rows.append((ap, n))
            return r0
        R_BADA = [add_rows(b_ada[l:l + 1, :].rearrange("o (r p) -> (o r) p", p=128), 48) for l in range(DEPTH)]
        R_C = add_rows(c_d.rearrange("o (r p) -> (o r) p", p=128), 8)
        R_CC = add_rows(cctx_d.rearrange("o (r p) -> (o r) p", p=128), 8)
        R_HLB = add_rows(hlb.rearrange("l d (r p) -> (l d r) p", p=128), 16)
        R_CW = add_rows(conv_w.rearrange("l t (r p) -> (l t r) p", p=128), 24)
        R_CB = add_rows(conv_b.rearrange("l (r p) -> (l r) p", p=128), 8)
        NR = sum(r for _, r in rows)
        assert NR <= 256
        with K.phase():
            stg = [K.sb("stg0", [128, 128], F32), K.sb("stg1", [128, 128], F32)]
            for s_ in stg:
                K.op("pool", lambda e: e.memset(s_[:], 0.0), wr=[s_])
            r0 = 0
            for ap, n in rows:
                done = 0
                while done < n:
                    g = (r0 + done) // 128
                    lo = (r0 + done) % 128
                    m = min(n - done, 128 - lo)
                    K.dma("sp", stg[g][lo:lo + m, :], ap[done:done + m, :], wr=stg[g])
                    done += m
                r0 += n
            for g in range(2):
                K.op("pe", lambda e: e.transpose(PB[g][:, 0:128], stg[g][:], identf[:]), rd=[stg[g], identf], wr=[PB[g]])
                cp(K, "dve", smT[:, 128 * g:128 * g + 128], PB[g][:, 0:128], rd=[PB[g]], wr=[smT])
            act(K, condT[:, :, 0], smT[:, R_C:R_C + 8], AF.Silu, rd=[smT], wr=[condT])
            act(K, condT[:, :, 1], smT[:, R_CC:R_CC + 8], AF.Silu, rd=[smT], wr=[condT])
            cp(K, "dve", condR[:].rearrange("p k s m -> p (k s) m"),
               condT[:].rearrange("p k s -> p (k s)").unsqueeze(2).to_broadcast([128, 16, 128]), rd=[condT], wr=[condR])
            hv = smT[:, R_HLB:R_HLB + 16].rearrange("p (l d r) -> p l d r", l=2, d=2)
            K.op("pool", lambda e: e.memset(lbT[:], 0.0), wr=[lbT])
            tt(K, "dve", lbT[:, 1], hv[:, 1], hv[:, 0], ALU.subtract, rd=[smT, lbT], wr=[lbT])
            act(K, lbT[:, 1], lbT[:, 1], AF.Sigmoid, rd=[lbT], wr=[lbT])
            ts(K, "dve", omlT[:], lbT[:], -1.0, 1.0, ALU.mult, ALU.add, rd=[lbT], wr=[omlT])

        def src_rows(l, gi):
            if l == 0:
                if gi < NCT:
                    return ctx_d[128 * gi:128 * gi + 128, :]
                return x_d[128 * (gi - NCT):128 * (gi - NCT) + 128, :]
            return X2[128 * gi:128 * gi + 128, :]

        stop_at(1)
        for l in range(DEPTH):
            last = l == DEPTH - 1
            with K.phase():
                modT = K.sb("modT", [128, 48, 2], F32)
                G = {}
                for j in (2, 5):
                    for st in range(2):
                        if last and st == 1:
                            continue
                        G[(j, st)] = K.sb(f"G{j}{st}", [128, D], F32)
                lnG = [K.sb("lng0", [128, D], F32)]
                lnB = [K.sb("lnb0", [128, D], F32)]
                normw = K.sb("normw", [128, D], F32)
                mgbB = K.sb("mgbB", [128, 16], F32)
                K.dma("sp", lnG[0][:], ln_g[l, 0:1, :].to_broadcast([128, D]), wr=lnG[0])
                K.dma("sp", lnB[0][:], ln_b[l, 0:1, :].to_broadcast([128, D]), wr=lnB[0])
                K.dma("sp", normw[:, 0:512], hg_norm[l:l + 1, :].to_broadcast([128, 512]), wr=normw)
                K.dma("sp", normw[:, 512:1024], ml_norm[l:l + 1, :].to_broadcast([128, 512]), wr=normw)
                K.dma("sp", mgbB[:], mgb[l:l + 1, :].to_broadcast([128, 16]), wr=mgbB)

                with K.phase():
                    wab = [K.sb(f"wab{i}", [128, 8, D], BF16) for i in range(2)]
                    bb = K.sb("bb", [128, D], F32)
                    wav = w_ada[l].rearrange("(k p) n -> p k n", p=128)
                    for j in range(6):
                        wb = wab[j % 2]
                        K.dma("pool", wb[:], wav[:, :, j * D:(j + 1) * D], wr=wb)
                        if j in (2, 5):
                            K.dma("sp", bb[:], b_ada[l:l + 1, j * D:(j + 1) * D].to_broadcast([128, D]), wr=bb)
                            for st in range(2):
                                if (j, st) not in G:
                                    continue
                                for hf in range(2):
                                    pz = PB[2 + hf]
                                    for kk in range(8):
                                        mm(K, pz[:, :], condR[:, kk, st, :], wb[:, kk, 512 * hf:512 * hf + 512],
                                           rd=[condR, wb], wr=[pz], start=kk == 0, stop=kk == 7)
                                    tt(K, "dve", G[(j, st)][:, 512 * hf:512 * hf + 512], pz[:, :],
                                       bb[:, 512 * hf:512 * hf + 512], ALU.add, rd=[pz, bb], wr=[G[(j, st)]])
                        else:
                            pz = PB[4]
                            for kc in range(8):
                                for kk in range(8):
                                    mm(K, pz[:, 2 * kc:2 * kc + 2], wb[:, kk, 128 * kc:128 * kc + 128], condT[:, kk, :],
                                       rd=[condT, wb], wr=[pz], start=kk == 0, stop=kk == 7)
                            tt(K, "dve", modT[:, 8 * j:8 * j + 8, :], pz[:, 0:16].rearrange("p (k s) -> p k s", s=2),
                               smT[:, R_BADA[l] + 8 * j:R_BADA[l] + 8 * j + 8].unsqueeze(2).to_broadcast([128, 8, 2]),
                               ALU.add, rd=[pz, smT], wr=[modT])
                    for j in (1, 4):
                        ts(K, "dve", modT[:, 8 * j:8 * j + 8, :], modT[:, 8 * j:8 * j + 8, :], 1.0, None, ALU.add, None,
                           rd=[modT], wr=[modT])

                stop_at(2)
                def macros(dirn):
                    ms = [(1, list(range(NCT)))]
                    for m in range(0, NLT, 4):
                        ms.append((0, [NCT + m + i for i in range(min(4, NLT - m))]))
                    if dirn == 1:
                        c0 = ms[0]
                        lat = ms[1:][::-1]
                        ms = [(c0[0], c0[1][::-1])] + [(s, t[::-1]) for s, t in lat]
                    return ms

                for dirn in (1, 0):
                    fwd = dirn == 0
                    with K.phase():
                        IJb = identb if fwd else revb
                        xt = [K.sb(f"xt{i}", [128, D], F32) for i in range(2)]
                        xb = [K.sb(f"xb{i}", [128, D], BF16) for i in range(1)]
                        hT = K.sb("hT", [128, 8, 512], BF16)
                        wblk = [K.sb(f"wblk{i}", [128, 8, 528], BF16) for i in range(2 if fwd else 3)]
                        qTh = K.sb("qTh", [128, 4, 512], BF16)
                        fbuf = K.sb("fbuf", [128, 4, 512], F32)
                        lfb = K.sb("lfb", [128, 4, 512], F32)
                        kTh = K.sb("kTh", [128, 4, 512], BF16)
                        qkT = K.sb("qkT", [128, 4, 512], BF16)
                        vhg = K.sb("vhg", [128, 4, 512], BF16)
                        vml = K.sb("vml", [128, 4, 4, 129], BF16)
                        gts_ = K.sb("gates", [128, 4, 16], F32)
                        Dall = K.sb("Dall", [128, 4, 576], F32)
                        QK = [K.sb(f"QK{i}", [128, 4, 576], BF16) for i in range(2)]
                        Asb = [K.sb(f"Asb{i}", [128, 4, 128], BF16) for i in range(2)]
                        khtm = K.sb("khtm", [128, 4, 128], BF16)
                        Sf = K.sb("Sf", [128, 4, 128], F32)
                        Sb = K.sb("Sb", [128, 4, 128], BF16)
                        tmpS = K.sb("tmpS", [128, 4, 128], F32)
                        Ssb = K.sb("Ssb", [128, 4, 128], BF16)
                        ktm = K.sb("ktm", [128, 4, 128], BF16)
                        Cf = K.sb("Cf", [128, 4, 129], F32)
                        Cb = K.sb("Cb", [128, 4, 129], BF16)
                        tmpC = K.sb("tmpC", [128, 4, 129], F32)
                        kTm = K.sb("kTm", [128, 4, 512], BF16)
                        sml = K.sb("sml", [128, 40], F32)
                        hl = K.sb("hl", [128, 2, 4], BF16)
                        lfm = K.sb("lfm", [128, 4, 4], F32)
                        hlM = K.sb("hlM", [128, 2, 4, 4], BF16)
                        smM = K.sb("smM", [128, 4, 16], F32)
                        obH = [K.sb(f"obH{i}", [128, 512], F32) for i in range(2)]
                        obM = [K.sb(f"obM{i}", [128, 512], F32) for i in range(2)]
                        decS = [K.sb(f"decS{i}", [128, 4, 1], F32) for i in range(2)]
                        if fwd:
                            gate = K.sb("gate", [128, 4, D], BF16)
                            wout = K.sb("wout", [128, 8, D], BF16)
                            ob = [K.sb(f"ob{i}", [128, D], F32) for i in range(2)]
                            xres = K.sb("xres", [128, D], F32)
                            sml2 = K.sb("sml2", [128, 16], F32)
                            ybf = K.sb("ybf", [128, D], BF16)
                            tmpA = K.sb("tmpA", [128, D], F32)
                            yT = K.sb("yT", [128, 8, 128], BF16)
                            lnscr = ln_scratch(K)
                            K.dma("pool", wout[:], w_out[l].rearrange("(k p) n -> p k n", p=128), wr=wout)
                        if os.environ.get("MK_VERBOSE"):
                            print("token-mix sbuf remaining", fwd, nc.sbuf_bytes_remaining)
                        for a_ in Asb:
                            K.op("pool", lambda e: e.memset(a_[:], 0.0), wr=[a_])
                        K.op("pool", lambda e: e.memset(vml[:], 1.0), wr=[vml])
                        K.op("pool", lambda e: e.memset(Sf[:], 0.0), wr=[Sf])
                        K.op("pool", lambda e: e.memset(Sb[:], 0.0), wr=[Sb])
                        K.op("pool", lambda e: e.memset(Cf[:], 0.0), wr=[Cf])
                        K.op("pool", lambda e: e.memset(Cb[:], 0.0), wr=[Cb])
                        K.op("pool", lambda e: e.memset(ktm[:], 0.0), wr=[ktm])
                        wiv = w_in[l].rearrange("(k p) n -> p k n", p=128)
                        cwo = R_CW + l * 12
                        cbo = R_CB + l * 4
                        cnt = {"xt": 0, "w": 0, "t": 0}

                        def step1_tile(st, gi, s):
                            if True:
                                xtt = xt[cnt["xt"] % 2]
                                xbt = xb[0]
                                cnt["xt"] += 1
                                K.dma("sp", xtt[:], src_rows(l, gi), wr=xtt)
                                cp(K, "act", xbt[:], xtt[:], rd=[xtt], wr=[xbt])
                                for hb in range(2):
                                    pt = PB[hb]
                                    for kq in range(4):
                                        k = 4 * hb + kq
                                        mm(K, pt[:, 128 * kq:128 * kq + 128], xbt[:, 128 * k:128 * k + 128], IJb[:],
                                           rd=[xbt, IJb], wr=[pt])
                                    for kq in range(4):
                                        k = 4 * hb + kq
                                        sc = modT[:, 8 + k, st:st + 1]
                                        sh = modT[:, k, st:st + 1]
                                        if kq % 2 == 0:
                                            act(K, hT[:, k, 128 * s:128 * s + 128], pt[:, 128 * kq:128 * kq + 128],
                                                AF.Identity, rd=[pt, modT], wr=[hT], bias=sh, scale=sc)
                                        else:
                                            ts(K, "dve", hT[:, k, 128 * s:128 * s + 128], pt[:, 128 * kq:128 * kq + 128],
                                               sc, sh, ALU.mult, ALU.add, rd=[pt, modT], wr=[hT])

                        def load_w(c0, ncols):
                            wbk = wblk[cnt["w"] % len(wblk)]
                            cnt["w"] += 1
                            K.dma("pool", wbk[:, :, 0:ncols], wiv[:, :, c0:c0 + ncols], wr=wbk)
                            return wbk

                        def prefetch_w():
                            return [load_w(0, 512), load_w(512 * (1 + dirn), 512)]

                        mlist = macros(dirn)
                        for s_, gi_ in enumerate(mlist[0][1]):
                            step1_tile(mlist[0][0], gi_, s_)
                        pre_w = prefetch_w()
                        for mi, (st, tiles) in enumerate(mlist):
                            nsl = len(tiles)
                            NT = 128 * nsl
                            nxt = mlist[mi + 1] if mi + 1 < len(mlist) else None
                            stop_at(3)

                            def proj_fm(wbk, c, evac):
                                pz = PB[2 + (cnt["t"] % 2)]
                                cnt["t"] += 1
                                for k in range(8):
                                    mm(K, pz[:, 0:NT], wbk[:, k, 128 * c:128 * c + 128], hT[:, k, 0:NT],
                                       rd=[wbk, hT], wr=[pz], start=k == 0, stop=k == 7)
                                evac(pz, c)

                            def proj_tm(wbk, s, ncols, evac):
                                pz = PB[2 + (cnt["t"] % 2)]
                                cnt["t"] += 1
                                for k in range(8):
                                    mm(K, pz[:, 0:ncols], hT[:, k, 128 * s:128 * s + 128], wbk[:, k, 0:ncols],
                                       rd=[wbk, hT], wr=[pz], start=k == 0, stop=k == 7)
                                evac(pz, s)

                            wbk = pre_w[0]
                            for c in range(4):
                                proj_fm(wbk, c, lambda pz, c: act(K, qTh[:, c, 0:NT], pz[:, 0:NT], AF.Silu, rd=[pz], wr=[qTh]))
                            wbk = pre_w[1]
                            for c in range(4):
                                def ev_f(pz, c):
                                    act(K, fbuf[:, c, 0:NT], pz[:, 0:NT], AF.Sigmoid, rd=[pz], wr=[fbuf])
                                    ts(K, "dve", fbuf[:, c, 0:NT], fbuf[:, c, 0:NT], omlT[:, l, dirn, c:c + 1],
                                       lbT[:, l, dirn, c:c + 1], ALU.mult, ALU.add, rd=[fbuf, omlT, lbT], wr=[fbuf])
                                proj_fm(wbk, c, ev_f)
                            act(K, lfb[:, :, 0:NT], fbuf[:, :, 0:NT], AF.Ln, rd=[fbuf], wr=[lfb])
                            act(K, kTh[:, :, 0:NT], fbuf[:, :, 0:NT], AF.Identity, rd=[fbuf], wr=[kTh], scale=-1.0, bias=1.0)
                            for c in range(4):
                                K.op("dve", lambda e: e.tensor_tensor_scan(
                                    out=fbuf[:, c, 0:NT], data0=rmask[:, 0:NT], data1=lfb[:, c, 0:NT], initial=0.0,
                                    op0=ALU.mult, op1=ALU.add), rd=[rmask, lfb], wr=[fbuf])
                            if fwd:
                                wbk = load_w(3584, 528)
                                for s in range(nsl):
                                    def ev_g(pz, s):
                                        tt(K, "dve", gts_[:, s, :], pz[:, 0:16], mgbB[:], ALU.add, rd=[pz, mgbB], wr=[gts_])
                                    proj_tm(wbk, s, 16, ev_g)
                                for s in range(nsl):
                                    pz = PB[2 + (cnt["t"] % 2)]
                                    cnt["t"] += 1
                                    for k in range(8):
                                        mm(K, pz[:, 0:512], hT[:, k, 128 * s:128 * s + 128], wbk[:, k, 16:528],
                                           rd=[wbk, hT], wr=[pz], start=k == 0, stop=k == 7)
                                    act(K, gate[:, s, 512:1024], pz[:, :], AF.Sigmoid, rd=[pz], wr=[gate])
                            else:
                                wbk = load_w(3584, 16)
                                for s in range(nsl):
                                    def ev_g(pz, s):
                                        tt(K, "dve", gts_[:, s, :], pz[:, 0:16], mgbB[:], ALU.add, rd=[pz, mgbB], wr=[gts_])
                                    proj_tm(wbk, s, 16, ev_g)

                            act(K, lfm[:, 0:nsl, :], gts_[:, 0:nsl, 8 * dirn + 4:8 * dirn + 8], AF.Sigmoid, rd=[gts_], wr=[lfm])
                            act(K, lfm[:, 0:nsl, :], lfm[:, 0:nsl, :], AF.Ln, rd=[lfm], wr=[lfm])
                            cp(K, "dve", hlM[:, 0, 0:nsl, :], lfm[:, 0:nsl, :], rd=[lfm], wr=[hlM])
                            tt(K, "dve", hlM[:, 1, 0:nsl, :], lfm[:, 0:nsl, :], hlM[:, 0, 0:nsl, :], ALU.subtract, rd=[lfm, hlM], wr=[hlM])
                            pbm = PB[3]
                            n4 = 4 * nsl
                            for i2 in range(2):
                                mm(K, pbm[:, 0:n4], trib[:], hlM[:, i2, 0:nsl, :].rearrange("p s h -> p (s h)"), rd=[trib, hlM], wr=[pbm],
                                   start=i2 == 0, stop=i2 == 1)
                            for i2 in range(2):
                                mm(K, pbm[:, 16:16 + n4], onesb[:], hlM[:, i2, 0:nsl, :].rearrange("p s h -> p (s h)"), rd=[onesb, hlM], wr=[pbm],
                                   start=i2 == 0, stop=i2 == 1)
                            bv = pbm[:, 0:n4].rearrange("p (s h) -> p s h", h=4)
                            btv = pbm[:, 16:16 + n4].rearrange("p (s h) -> p s h", h=4)
                            tt(K, "dve", smM[:, 0:nsl, 4:8], gts_[:, 0:nsl, 8 * dirn:8 * dirn + 4], bv, ALU.subtract, rd=[gts_, pbm], wr=[smM])
                            act(K, smM[:, 0:nsl, 4:8], smM[:, 0:nsl, 4:8], AF.Exp, rd=[smM], wr=[smM])
                            act(K, smM[:, 0:nsl, 8:12], bv, AF.Exp, rd=[pbm], wr=[smM], scale=-1.0, bias=math.log(8.0))
                            act(K, smM[:, 0:nsl, 12:16], btv, AF.Exp, rd=[pbm], wr=[smM])
                            wbk = load_w(1536, 512)
                            for s in range(nsl):
                                proj_tm(wbk, s, 512, lambda pz, s: cp(K, "act", vhg[:, s, :], pz[:, :], rd=[pz], wr=[vhg]))
                            if fwd:
                                wbk = load_w(2048, 512)
                                for s in range(nsl):
                                    proj_tm(wbk, s, 512, lambda pz, s: act(K, gate[:, s, 0:512], pz[:, :], AF.Silu,
                                                                           rd=[pz], wr=[gate]))
                            wbk = load_w(2560, 512)
                            rw = 64 if st == 0 else NT
                            wp, wn = (0, 2) if fwd else (2, 0)
                            for c in range(4):
                                def ev_c(pz, c):
                                    cw = lambda tap: smT[:, cwo + 4 * tap + c:cwo + 4 * tap + c + 1]
                                    cv = lfb[:, c, 0:NT]
                                    ts(K, "dve", cv, pz[:, 0:NT], cw(1), smT[:, cbo + c:cbo + c + 1], ALU.mult, ALU.add,
                                       rd=[pz, smT], wr=[lfb])
                                    cv3 = cv.rearrange("p (r j) -> p r j", j=rw)
                                    pz3 = pz[:, 0:NT].rearrange("p (r j) -> p r j", j=rw)
                                    stt(K, cv3[:, :, 1:rw], pz3[:, :, 0:rw - 1], cw(wp), cv3[:, :, 1:rw], ALU.mult, ALU.add,
                                        rd=[pz, smT, lfb], wr=[lfb])
                                    stt(K, cv3[:, :, 0:rw - 1], pz3[:, :, 1:rw], cw(wn), cv3[:, :, 0:rw - 1], ALU.mult, ALU.add,
                                        rd=[pz, smT, lfb], wr=[lfb])
                                proj_fm(wbk, c, ev_c)
                            act(K, qkT[:, :, 0:NT], lfb[:, :, 0:NT], AF.Silu, rd=[lfb], wr=[qkT])
                            for h in range(4):
                                if h % 2:
                                    act(K, kTm[:, h, 0:NT], qkT[:, 2 + h // 2, 0:NT], AF.Copy, rd=[qkT, hmask], wr=[kTm],
                                        scale=hmask[:, h % 2:h % 2 + 1])
                                else:
                                    ts(K, "dve", kTm[:, h, 0:NT], qkT[:, 2 + h // 2, 0:NT], hmask[:, h % 2:h % 2 + 1], None,
                                       ALU.mult, None, rd=[qkT, hmask], wr=[kTm])
                            wbk = load_w(3072, 512)
                            for s in range(nsl):
                                proj_tm(wbk, s, 512, lambda pz, s: cp(K, "act", vml[:, s, :, 0:128],
                                                                      pz[:, :].rearrange("p (h e) -> p h e", e=128),
                                                                      rd=[pz], wr=[vml]))
                            stop_at(4)
                            need_out = not (last and st == 1)
                            offs = {0: 0, 1: 128, 2: 224, 3: 288}

                            def lane_hgA(s):
                                tsl = slice(128 * s, 128 * s + 128)
                                qk = QK[s % 2]
                                g_ = fbuf[:, :, tsl]
                                cp(K, "act", Dall[:, :, 0:128], g_, rd=[fbuf], wr=[Dall])
                                for a in (1, 2, 3):
                                    n = 128 - 32 * a
                                    tt(K, "dve", Dall[:, :, offs[a]:offs[a] + n], g_[:, :, 32 * a:128],
                                       g_[:, :, 32 * a - 1:32 * a].to_broadcast([128, 4, n]), ALU.subtract, rd=[fbuf], wr=[Dall])
                                    tt(K, "dve", Dall[:, :, 320 + 32 * a:352 + 32 * a],
                                       g_[:, :, 32 * a - 1:32 * a].to_broadcast([128, 4, 32]), g_[:, :, 32 * a:32 * a + 32],
                                       ALU.subtract, rd=[fbuf], wr=[Dall])
                                act(K, Dall[:, :, 320:352], g_[:, :, 0:32], AF.Copy, rd=[fbuf], wr=[Dall], scale=-1.0)
                                tt(K, "dve", Dall[:, :, 448:576], g_[:, :, 127:128].to_broadcast([128, 4, 128]), g_,
                                   ALU.subtract, rd=[fbuf], wr=[Dall])
                                act(K, Dall[:], Dall[:], AF.Exp, rd=[Dall], wr=[Dall])
                                for a in range(4):
                                    n = 128 - 32 * a
                                    tt(K, "dve", qk[:, :, offs[a]:offs[a] + n],
                                       qTh[:, :, 128 * s + 32 * a:128 * s + 128], Dall[:, :, offs[a]:offs[a] + n], ALU.mult,
                                       rd=[qTh, Dall], wr=[qk])
                                tt(K, "dve", qk[:, :, 320:448], kTh[:, :, tsl], Dall[:, :, 320:448], ALU.mult, rd=[kTh, Dall], wr=[qk])
                                tt(K, "dve", qk[:, :, 448:576], kTh[:, :, tsl], Dall[:, :, 448:576], ALU.mult, rd=[kTh, Dall], wr=[qk])
                                cp(K, "dve", decS[s % 2][:], Dall[:, :, 127:128], rd=[Dall], wr=[decS[s % 2]])

                            def lane_hgB(s, gi):
                                tsl = slice(128 * s, 128 * s + 128)
                                qk = QK[s % 2]
                                asb = Asb[s % 2]
                                obh = obH[s % 2]
                                pa = PB[4]
                                for h in range(4):
                                    for a in range(4):
                                        n = 128 - 32 * a
                                        mm(K, pa[32 * a:32 * a + 32, 128 * h + 32 * a:128 * h + 128],
                                           qk[:, h, 320 + 32 * a:352 + 32 * a], qk[:, h, offs[a]:offs[a] + n],
                                           rd=[qk], wr=[pa], tp=(0, 32 * a))
                                pa3 = pa[:, :].rearrange("p (h t) -> p h t", t=128)
                                for a in range(4):
                                    tt(K, "dve", asb[32 * a:32 * a + 32, :, 32 * a:128], pa3[32 * a:32 * a + 32, :, 32 * a:128],
                                       mask4[32 * a:32 * a + 32, :, 32 * a:128], ALU.mult, rd=[pa, mask4], wr=[asb])
                                pk = PB[5]
                                for h in range(4):
                                    mm(K, pk[:, 128 * h:128 * h + 128], qk[:, h, 448:576], identb[:], rd=[qk, identb], wr=[pk])
                                cp(K, "act", khtm[:].rearrange("p h d -> p (h d)"), pk[:, :], rd=[pk], wr=[khtm])
                                po = PB[6]
                                if need_out:
                                    first = True
                                    if fwd:
                                        obt = ob[s % 2]
                                        mm(K, po[:, :], revf[:], obt[:, 0:512], rd=[revf, obt], wr=[po], start=True, stop=False)
                                        first = False
                                    for h in range(4):
                                        hs = slice(128 * h, 128 * h + 128)
                                        mm(K, po[:, hs], asb[:, h, :], vhg[:, s, hs], rd=[asb, vhg], wr=[po],
                                           start=first, stop=False)
                                        mm(K, po[:, hs], qk[:, h, 0:128], Sb[:, h, :], rd=[qk, Sb], wr=[po],
                                           start=False, stop=(h == 3) if fwd else True)
                                pu = PB[4]
                                for h in range(4):
                                    hs = slice(128 * h, 128 * h + 128)
                                    mm(K, pu[:, hs], khtm[:, h, :], vhg[:, s, hs], rd=[khtm, vhg], wr=[pu])
                                tt(K, "dve", tmpS[:], Sf[:], decS[s % 2][:].to_broadcast([128, 4, 128]), ALU.mult,
                                   rd=[Sf, decS[s % 2]], wr=[tmpS])
                                tt(K, "dve", Sf[:], tmpS[:], pu[:, :].rearrange("p (h e) -> p h e", e=128), ALU.add,
                                   rd=[tmpS, pu], wr=[Sf])
                                cp(K, "act", Sb[:], Sf[:], rd=[Sf], wr=[Sb])
                                if need_out:
                                    cp(K, "act", obh[:], po[:, :], rd=[po], wr=[obh])
                                    if not fwd:
                                        K.dma("sp", OB[128 * gi:128 * gi + 128, 0:512], obh[:], rd=obh)

                            def lane_ml(s, gi):
                                tsl = slice(128 * s, 128 * s + 128)
                                obm = obM[s % 2]
                                pb = PB[3]
                                psS = PB[7]
                                for h in range(4):
                                    g = h // 2
                                    mm(K, psS[:, 128 * h:128 * h + 128], kTm[:, h, tsl], qkT[:, g, tsl], rd=[kTm, qkT], wr=[psS])
                                for h in range(4):
                                    stt(K, Ssb[:, h, :], psS[:, 128 * h:128 * h + 128], smM[:, s, 4 + h:5 + h], mask4[:, h, :],
                                        ALU.mult, ALU.mult, rd=[psS, smM, mask4], wr=[Ssb])
                                for g in range(2):
                                    mm(K, pb[:, 16 + 128 * g:144 + 128 * g], qkT[:, 2 + g, tsl], identb[:], rd=[qkT, identb], wr=[pb])
                                pbk = pb[:, 16:272].rearrange("p (g h d) -> p g h d", g=2, h=2)
                                ktv = ktm[:].rearrange("p (g h) (x d) -> p g h x d", h=2, x=2)
                                for hp in range(2):
                                    tt(K, "dve", ktv[:, :, hp, hp, :], pbk[:, :, hp, :],
                                       smM[:, s, 4 + hp:8:2].unsqueeze(2).to_broadcast([128, 2, 64]), ALU.mult, rd=[pb, smM], wr=[ktm])
                                pn = PB[7]
                                pd = PB[3]
                                if need_out:
                                    for h in range(4):
                                        g = h // 2
                                        hs = slice(128 * h, 128 * h + 128)
                                        mm(K, pn[:, hs], Ssb[:, h, :], vml[:, s, h, 0:128], rd=[Ssb, vml], wr=[pn], start=True, stop=False)
                                        mm(K, pn[:, hs], qkT[:, g, tsl], Cb[:, h, 0:128], rd=[qkT, Cb], wr=[pn],
                                           start=False, stop=True)
                                        mm(K, pd[:, 8 + h:9 + h], Ssb[:, h, :], vml[:, s, h, 128:129], rd=[Ssb, vml], wr=[pd],
                                           start=True, stop=False)
                                        mm(K, pd[:, 8 + h:9 + h], qkT[:, g, tsl], Cb[:, h, 128:129],
                                           rd=[qkT, Cb], wr=[pd], start=False, stop=True)
                                    tt(K, "dve", sml[:, 16:20], pd[:, 8:12], smM[:, s, 8:12], ALU.max, rd=[pd, smM], wr=[sml])
                                    stt(K, sml[:, 16:20], pd[:, 8:12], -1.0, sml[:, 16:20], ALU.mult, ALU.max, rd=[pd, sml], wr=[sml])
                                    K.op("dve", lambda e: e.reciprocal(out=sml[:, 20:24], in_=sml[:, 16:20]), rd=[sml], wr=[sml])
                                    tt(K, "dve", obm[:].rearrange("p (h e) -> p h e", e=128),
                                       pn[:, :].rearrange("p (h e) -> p h e", e=128),
                                       sml[:, 20:24].unsqueeze(2).to_broadcast([128, 4, 128]), ALU.mult, rd=[pn, sml], wr=[obm])
                                    if not fwd:
                                        K.dma("sp", OB[128 * gi:128 * gi + 128, 512:1024], obm[:], rd=obm)
                                for g in range(2):
                                    pc = PB[7] if g == 0 else PB[3]
                                    for hp in range(2):
                                        h = 2 * g + hp
                                        mm(K, pc[:, 129 * hp:129 * hp + 129], ktm[:, h, :], vml[:, s, h, :], rd=[ktm, vml], wr=[pc])
                                    tt(K, "dve", tmpC[:, 2 * g:2 * g + 2, :], Cf[:, 2 * g:2 * g + 2, :],
                                       pc[:, 0:258].rearrange("p (g e) -> p g e", e=129), ALU.add, rd=[Cf, pc], wr=[tmpC])
                                tt(K, "dve", Cf[:], tmpC[:], smM[:, s, 12:16].unsqueeze(2).to_broadcast([128, 4, 129]), ALU.mult,
                                   rd=[tmpC, smM], wr=[Cf])
                                cp(K, "act", Cb[:], Cf[:], rd=[Cf], wr=[Cb])

                            def lane_epi(s, gi):
                                obh, obm, obt = obH[s % 2], obM[s % 2], ob[s % 2]
                                pj = PB[2]
                                mm(K, pj[:, :], revf[:], obt[:, 512:1024], rd=[revf, obt], wr=[pj])
                                tt(K, "dve", obm[:], obm[:], pj[:, :], ALU.add, rd=[obm, pj], wr=[obm])
                                tt(K, "dve", tmpA[:, 0:512], obh[:], obh[:], ALU.mult, rd=[obh], wr=[tmpA])
                                tt(K, "dve", tmpA[:, 512:1024], obm[:], obm[:], ALU.mult, rd=[obm], wr=[tmpA])
                                K.op("dve", lambda e: e.tensor_reduce(out=sml2[:, 0:8], in_=tmpA[:].rearrange("p (h e) -> p h e", e=128),
                                                                      axis=AX.X, op=ALU.add), rd=[tmpA], wr=[sml2])
                                act(K, sml2[:, 0:8], sml2[:, 0:8], AF.Ln, rd=[sml2], wr=[sml2], scale=1.0 / 128.0, bias=RMS_EPS)
                                act(K, sml2[:, 8:16], sml2[:, 0:8], AF.Exp, rd=[sml2], wr=[sml2], scale=-0.5)
                                tt(K, "dve", tmpA[:, 0:512].rearrange("p (h e) -> p h e", e=128), obh[:].rearrange("p (h e) -> p h e", e=128),
                                   sml2[:, 8:12].unsqueeze(2).to_broadcast([128, 4, 128]), ALU.mult, rd=[obh, sml2], wr=[tmpA])
                                tt(K, "dve", tmpA[:, 512:1024].rearrange("p (h e) -> p h e", e=128), obm[:].rearrange("p (h e) -> p h e", e=128),
                                   sml2[:, 12:16].unsqueeze(2).to_broadcast([128, 4, 128]), ALU.mult, rd=[obm, sml2], wr=[tmpA])
                                tt(K, "dve", tmpA[:], tmpA[:], gate[:, s, :], ALU.mult, rd=[tmpA, gate], wr=[tmpA])
                                tt(K, "dve", ybf[:], tmpA[:], normw[:], ALU.mult, rd=[tmpA, normw], wr=[ybf])
                                for hb in range(2):
                                    pt = PB[hb]
                                    for kq in range(4):
                                        k = 4 * hb + kq
                                        mm(K, pt[:, 128 * kq:128 * kq + 128], ybf[:, 128 * k:128 * k + 128], identb[:],
                                           rd=[ybf, identb], wr=[pt])
                                    cp(K, "act" if hb == 0 else "dve", yT[:, 4 * hb:4 * hb + 4, :].rearrange("p k t -> p (k t)"),
                                       pt[:, :], rd=[pt], wr=[yT])
                                K.dma("sp", xres[:], src_rows(l, gi), wr=xres)
                                for hf in range(2):
                                    pz = PB[hf]
                                    cs = slice(512 * hf, 512 * hf + 512)
                                    for k in range(8):
                                        mm(K, pz[:, :], yT[:, k, :], wout[:, k, cs], rd=[yT, wout], wr=[pz], start=k == 0, stop=k == 7)
                                    tt(K, "dve", tmpA[:, cs], pz[:, :], G[(2, st)][:, cs], ALU.mult, rd=[pz, G[(2, st)]], wr=[tmpA])
                                stt(K, tmpA[:], xres[:], ALPHA, tmpA[:], ALU.mult, ALU.add, rd=[xres, tmpA], wr=[tmpA])
                                layer_norm(K, tmpA, tmpA, lnG[0], lnB[0], lnscr)
                                K.dma("sp", X1[128 * gi:128 * gi + 128, :], tmpA[:], rd=tmpA)

                            do_epi = fwd and need_out
                            if nxt is not None:
                                pre_w = prefetch_w()
                            nnx = len(nxt[1]) if nxt is not None else 0
                            if not os.environ.get("MK_LANE1") and nxt is not None:
                                for s_, gi_ in enumerate(nxt[1]):
                                    step1_tile(nxt[0], gi_, s_)
                                nnx = 0
                            lane_hgA(0)
                            for s in range(max(nsl + (1 if do_epi else 0), nnx)):
                                fns = []
                                if s < nsl:
                                    gi = tiles[s]
                                    if do_epi:
                                        K.dma("sp", ob[s % 2][:], OB[128 * gi:128 * gi + 128, :], wr=ob[s % 2])
                                    fns.append(lambda s=s, gi=gi: lane_hgB(s, gi))
                                    if s + 1 < nsl:
                                        fns.append(lambda s=s: lane_hgA(s + 1))
                                    fns.append(lambda s=s, gi=gi: lane_ml(s, gi))
                                def lane_c(s=s):
                                    if do_epi and 1 <= s <= nsl:
                                        lane_epi(s - 1, tiles[s - 1])
                                    if s < nnx:
                                        step1_tile(nxt[0], nxt[1][s], s)
                                fns.append(lane_c)
                                run_lanes(K, fns)
                                stop_at(6)

                    stop_at(7 + (1 - dirn))
                with K.phase():
                    if not last:
                        ffn_dense(K, nc, PB, identf, modT, G, ln_g[l, 1:2, :], ln_b[l, 1:2, :], X1, X2, ffn_gu, ffn_dn, NG, NCT)
                    else:
                        ffn_moe(K, nc, PB, identf, onesf, modT, G, ln_g[l, 1:2, :], ln_b[l, 1:2, :], X1, out_d, router_w, router_b,
                                moe_gu, moe_dn, NG, NCT)
            stop_at(9 + l)
        K.barrier()
    return nc


def ln_scratch(K):
    return (K.sb("st6", [128, 2, 6], F32), K.sb("mv2", [128, 2], F32))


def layer_norm(K, src, dst, g, b, scr):
    st6, mv = scr
    for i in range(2):
        K.op("dve", lambda e: e.bn_stats(out=st6[:, i, :], in_=src[:, 512 * i:512 * i + 512]), rd=[src], wr=[st6])
    K.op("dve", lambda e: e.bn_aggr(out=mv[:], in_=st6[:].rearrange("p a b -> p (a b)")), rd=[st6], wr=[mv])
    if os.environ.get("MK_LNSQRT"):
        act(K, mv[:, 1:2], mv[:, 1:2], AF.Sqrt, rd=[mv], wr=[mv], bias=LN_EPS)
        K.op("dve", lambda e: e.reciprocal(out=mv[:, 1:2], in_=mv[:, 1:2]), rd=[mv], wr=[mv])
    else:
        act(K, mv[:, 1:2], mv[:, 1:2], AF.Ln, rd=[mv], wr=[mv], bias=LN_EPS)
        act(K, mv[:, 1:2], mv[:, 1:2], AF.Exp, rd=[mv], wr=[mv], scale=-0.5)
    ts(K, "dve", src[:], src[:], mv[:, 0:1], mv[:, 1:2], ALU.subtract, ALU.mult, rd=[src, mv], wr=[src])
    tt(K, "dve", src[:], src[:], g[:], ALU.mult, rd=[src, g], wr=[src])
    tt(K, "dve", dst[:], src[:], b[:], ALU.add, rd=[src, b], wr=[dst])


def ffn_load_h(K, PB, identf, modT, X1, gi, st, xt, hT32, hT, s):
    K.dma("sp", xt[:], X1[128 * gi:128 * gi + 128, :], wr=xt)
    for hb in range(2):
        pt = PB[hb]
        for kq in range(4):
            k = 4 * hb + kq
            K.op("pe", lambda e: e.transpose(pt[:, 128 * kq:128 * kq + 128], xt[:, 128 * k:128 * k + 128], identf[:]),
                 rd=[xt, identf], wr=[pt])
        for kq in range(4):
            k = 4 * hb + kq
            sc = modT[:, 32 + k, st:st + 1]
            sh = modT[:, 24 + k, st:st + 1]
            dst = hT32[:, k, :] if hT32 is not None else hT[:, k, 128 * s:128 * s + 128]
            if kq % 2 == 0:
                act(K, dst, pt[:, 128 * kq:128 * kq + 128], AF.Identity, rd=[pt, modT], wr=[hT32 if hT32 is not None else hT],
                    bias=sh, scale=sc)
            else:
                ts(K, "dve", dst, pt[:, 128 * kq:128 * kq + 128], sc, sh, ALU.mult, ALU.add, rd=[pt, modT],
                   wr=[hT32 if hT32 is not None else hT])
    if hT32 is not None:
        cp(K, "act", hT[:, :, 128 * s:128 * s + 128], hT32[:], rd=[hT32], wr=[hT])


def ffn_dense(K, nc, PB, identf, modT, G, lng_d, lnb_d, X1, X2, ffn_gu, ffn_dn, NG, NCT):
    NCH = D_FF // 128
    lng = K.sb("flng", [128, D], F32)
    lnb = K.sb("flnb", [128, D], F32)
    K.dma("sp", lng[:], lng_d.to_broadcast([128, D]), wr=lng)
    K.dma("sp", lnb[:], lnb_d.to_broadcast([128, D]), wr=lnb)
    xt = [K.sb(f"fxt{i}", [128, D], F32) for i in range(2)]
    xrs = [K.sb(f"fxr{i}", [128, D], F32) for i in range(2)]
    hTs = [K.sb(f"fhT{i}", [128, 8, 512], BF16) for i in range(2)]
    wgu = [K.sb(f"wgu{i}", [128, 8, 2, 256], BF16) for i in range(3)]
    hid = K.sb("hid", [128, NCH, 512], BF16)
    sg = [K.sb(f"sg{i}", [128, 512], F32) for i in range(2)]
    wdn = [K.sb(f"wdn{i}", [128, NCH, 512], BF16) for i in range(2)]
    tmpA = K.sb("ftmpA", [128, D], F32)
    xo = K.sb("fxo", [128, D], F32)
    sml = ln_scratch(K)
    guv = ffn_gu[0].rearrange("(k p) n -> p k n", p=128)
    dnv = ffn_dn[0].rearrange("(c p) n -> p c n", p=128)
    macs = [(1, list(range(NCT)))]
    for m in range(NCT, NG, 4):
        macs.append((0, list(range(m, min(m + 4, NG)))))
    cntw = 0
    for hf in range(2):
        K.dma("pool", wdn[hf][:], dnv[:, :, 512 * hf:512 * hf + 512], wr=wdn[hf])

    def load_macro(mi):
        st_, tiles_ = macs[mi]
        for s_, gi_ in enumerate(tiles_):
            ffn_load_h(K, PB, identf, modT, X1, gi_, st_, xt[s_ % 2], None, hTs[mi % 2], s_)

    load_macro(0)
    for mi, (st, tiles) in enumerate(macs):
        nsl = len(tiles)
        NT = 128 * nsl
        hT = hTs[mi % 2]
        for blk in range(NCH // 2):
            wb = wgu[cntw % 3]
            cntw += 1
            K.dma("pool", wb[:, :, 0, :], guv[:, :, 256 * blk:256 * blk + 256], wr=wb)
            K.dma("pool", wb[:, :, 1, :], guv[:, :, D_FF + 256 * blk:D_FF + 256 * blk + 256], wr=wb)
            for cc in range(2):
                c = 2 * blk + cc
                pg, pu = PB[2 + (c % 2) * 2], PB[3 + (c % 2) * 2]
                for k in range(8):
                    mm(K, pg[:, 0:NT], wb[:, k, 0, 128 * cc:128 * cc + 128], hT[:, k, 0:NT], rd=[wb, hT], wr=[pg],
                       start=k == 0, stop=k == 7)
                for k in range(8):
                    mm(K, pu[:, 0:NT], wb[:, k, 1, 128 * cc:128 * cc + 128], hT[:, k, 0:NT], rd=[wb, hT], wr=[pu],
                       start=k == 0, stop=k == 7)
                sgt = sg[c % 2]
                act(K, sgt[:, 0:NT], pg[:, 0:NT], AF.Silu, rd=[pg], wr=[sgt])
                tt(K, "dve", hid[:, c, 0:NT], sgt[:, 0:NT], pu[:, 0:NT], ALU.mult, rd=[sgt, pu], wr=[hid])
        if mi + 1 < len(macs):
            load_macro(mi + 1)
        for s, gi in enumerate(tiles):
            for hf in range(2):
                pz = PB[6 + hf]
                cs = slice(512 * hf, 512 * hf + 512)
                for c in range(NCH):
                    mm(K, pz[:, :], hid[:, c, 128 * s:128 * s + 128], wdn[hf][:, c, :], rd=[hid, wdn[hf]], wr=[pz],
                       start=c == 0, stop=c == NCH - 1)
                tt(K, "dve", tmpA[:, cs], pz[:, :], G[(5, st)][:, cs], ALU.mult, rd=[pz, G[(5, st)]], wr=[tmpA])
            xr = xrs[s % 2]
            K.dma("sp", xr[:], X1[128 * gi:128 * gi + 128, :], wr=xr)
            stt(K, tmpA[:], xr[:], ALPHA, tmpA[:], ALU.mult, ALU.add, rd=[xr, tmpA], wr=[tmpA])
            layer_norm(K, tmpA, xo, lng, lnb, sml)
            K.dma("sp", X2[128 * gi:128 * gi + 128, :], xo[:], rd=xo)


def ffn_moe(K, nc, PB, identf, onesf, modT, G, lng_d, lnb_d, X1, out_d, router_w, router_b, moe_gu, moe_dn, NG, NCT):
    NCH = D_EXP // 128
    lng = K.sb("mlng", [128, D], F32)
    lnb = K.sb("mlnb", [128, D], F32)
    K.dma("sp", lng[:], lng_d.to_broadcast([128, D]), wr=lng)
    K.dma("sp", lnb[:], lnb_d.to_broadcast([128, D]), wr=lnb)
    NLT = NG - NCT
    MT = 8 if NLT % 8 == 0 else 4
    xt = [K.sb(f"mxt{i}", [128, D], F32) for i in range(2)]
    hTs = [K.sb(f"mhT{i}", [128, 8, 128 * MT], BF16) for i in range(2)]
    hT32 = K.sb("mhT32", [128, 8, 128], F32)
    rw = K.sb("rw", [128, 8, NE], F32)
    rwh = K.sb("rwh", [128, 8, NE], BF16)
    rwl = K.sb("rwl", [128, 8, NE], BF16)
    hlo = K.sb("hlo", [128, 8, 128], BF16)
    rbB = K.sb("rbB", [128, NE], F32)
    combs = [K.sb(f"comb{i}", [128, MT, NE], F32) for i in range(2)]
    lg = K.sb("lg", [128, 32], F32)
    wgu = [K.sb(f"mwgu{i}", [128, 8, 2, 128], BF16) for i in range(3)]
    hid = K.sb("mhid", [128, NCH, 128 * MT], BF16)
    sg = [K.sb(f"msg{i}", [128, 512], F32) for i in range(2)]
    wdn = [K.sb(f"mwdn{i}", [128, NCH, D], BF16) for i in range(2)]
    acc = K.sb("macc", [128, MT, D], F32)
    tmpA = K.sb("mtmpA", [128, D], F32)
    lnscr = ln_scratch(K)
    if os.environ.get("MK_VERBOSE"):
        print("moe sbuf remaining", nc.sbuf_bytes_remaining)
    K.dma("sp", rw[:], router_w[0].rearrange("(k p) e -> p k e", p=128), wr=rw)
    K.dma("sp", rbB[:], router_b[0:1, :].to_broadcast([128, NE]), wr=rbB)
    cp(K, "dve", rwh[:], rw[:], rd=[rw], wr=[rwh])
    tt(K, "dve", rwl[:], rw[:], rwh[:], ALU.subtract, rd=[rw, rwh], wr=[rwl])
    cntw = 0
    mstarts = list(range(NCT, NG, MT))
    xl = [xt[0]]

    def load_macro(mi):
        hT = hTs[mi % 2]
        comb = combs[mi % 2]
        for s in range(MT):
            gi = mstarts[mi] + s
            ffn_load_h(K, PB, identf, modT, X1, gi, 0, xl[0], hT32, hT, s)
            pr = PB[2]
            hs_ = slice(128 * s, 128 * s + 128)
            tt(K, "dve", hlo[:], hT32[:], hT[:, :, hs_], ALU.subtract, rd=[hT32, hT], wr=[hlo])
            for k in range(8):
                mm(K, pr[:, 0:NE], hT[:, k, hs_], rwh[:, k, :], rd=[hT, rwh], wr=[pr], start=k == 0, stop=False)
                mm(K, pr[:, 0:NE], hlo[:, k, :], rwh[:, k, :], rd=[hlo, rwh], wr=[pr], start=False, stop=False)
                mm(K, pr[:, 0:NE], hT[:, k, hs_], rwl[:, k, :], rd=[hT, rwl], wr=[pr], start=False, stop=k == 7)
            tt(K, "dve", lg[:, 0:8], pr[:, 0:NE], rbB[:], ALU.add, rd=[pr, rbB], wr=[lg])
            K.op("dve", lambda e: e.max(out=lg[:, 8:16], in_=lg[:, 0:8]), rd=[lg], wr=[lg])
            ts(K, "dve", lg[:, 16:24], lg[:, 0:8], lg[:, 8:9], None, ALU.subtract, None, rd=[lg], wr=[lg])
            act(K, lg[:, 16:24], lg[:, 16:24], AF.Exp, rd=[lg], wr=[lg])
            ts(K, "dve", lg[:, 24:32], lg[:, 0:8], lg[:, 9:10], None, ALU.is_ge, None, rd=[lg], wr=[lg])
            tt(K, "dve", lg[:, 16:24], lg[:, 16:24], lg[:, 24:32], ALU.mult, rd=[lg], wr=[lg])
            K.op("dve", lambda e: e.tensor_reduce(out=lg[:, 24:25], in_=lg[:, 16:24], axis=AX.X, op=ALU.add), rd=[lg], wr=[lg])
            K.op("dve", lambda e: e.reciprocal(out=lg[:, 25:26], in_=lg[:, 24:25]), rd=[lg], wr=[lg])
            ts(K, "dve", comb[:, s, :], lg[:, 16:24], lg[:, 25:26], None, ALU.mult, None, rd=[lg], wr=[comb])

    PIPE = bool(os.environ.get("MK_MOEPIPE"))
    if PIPE:
        load_macro(0)
    for mi, m0 in enumerate(mstarts):
        if not PIPE:
            load_macro(mi)
        tiles = list(range(m0, m0 + MT))
        NT = 128 * MT
        NH = NT // 512
        hT = hTs[mi % 2]
        comb = combs[mi % 2]
        for ex in range(NE):
            guv = moe_gu[0, ex].rearrange("(k p) n -> p k n", p=128)
            dnv = moe_dn[0, ex].rearrange("(c p) n -> p c n", p=128)
            wd = wdn[ex % 2]
            K.dma("pool", wd[:], dnv, wr=wd)
            for c in range(NCH):
                wb = wgu[cntw % 3]
                cntw += 1
                K.dma("pool", wb[:, :, 0, :], guv[:, :, 128 * c:128 * c + 128], wr=wb)
                K.dma("pool", wb[:, :, 1, :], guv[:, :, D_EXP + 128 * c:D_EXP + 128 * c + 128], wr=wb)
                for hf in range(NH):
                    ts_ = slice(512 * hf, 512 * hf + 512)
                    pg, pu = PB[2 + (hf % 2) * 2], PB[3 + (hf % 2) * 2]
                    for k in range(8):
                        mm(K, pg[:, :], wb[:, k, 0, :], hT[:, k, ts_], rd=[wb, hT], wr=[pg], start=k == 0, stop=k == 7)
                    for k in range(8):
                        mm(K, pu[:, :], wb[:, k, 1, :], hT[:, k, ts_], rd=[wb, hT], wr=[pu], start=k == 0, stop=k == 7)
                    sgt = sg[hf % 2]
                    act(K, sgt[:], pg[:, :], AF.Silu, rd=[pg], wr=[sgt])
                    tt(K, "dve", hid[:, c, ts_], sgt[:], pu[:, :], ALU.mult, rd=[sgt, pu], wr=[hid])
            if PIPE and ex == NE - 1 and mi + 1 < len(mstarts):
                load_macro(mi + 1)
            for s in range(MT):
                for hf in range(2):
                    pz = PB[hf]
                    cs = slice(512 * hf, 512 * hf + 512)
                    for c in range(NCH):
                        mm(K, pz[:, :], hid[:, c, 128 * s:128 * s + 128], wd[:, c, cs], rd=[hid, wd], wr=[pz],
                           start=c == 0, stop=c == NCH - 1)
                    if ex == 0:
                        ts(K, "dve", acc[:, s, cs], pz[:, :], comb[:, s, ex:ex + 1], None, ALU.mult, None, rd=[pz, comb], wr=[acc])
                    else:
                        stt(K, acc[:, s, cs], pz[:, :], comb[:, s, ex:ex + 1], acc[:, s, cs], ALU.mult, ALU.add,
                            rd=[pz, comb, acc], wr=[acc])
        for s, gi in enumerate(tiles):
            tt(K, "dve", tmpA[:], acc[:, s, :], G[(5, 0)][:], ALU.mult, rd=[acc, G[(5, 0)]], wr=[tmpA])
            xr = xt[1]
            K.dma("sp", xr[:], X1[128 * gi:128 * gi + 128, :], wr=xr)
            stt(K, tmpA[:], xr[:], ALPHA, tmpA[:], ALU.mult, ALU.add, rd=[xr, tmpA], wr=[tmpA])
            layer_norm(K, tmpA, tmpA, lng, lnb, lnscr)
            K.dma("sp", out_d[128 * (gi - NCT):128 * (gi - NCT) + 128, :], tmpA[:], rd=tmpA)


_CACHE = {}


def _core_inputs(inputs, b):
    f = lambda a: np.ascontiguousarray(np.asarray(a, dtype=np.float32))
    m = {
        "x": f(inputs["x"][b]),
        "c": f(inputs["c"][b:b + 1]),
        "ctx": f(inputs["ctx"][b]),
        "c_ctx": f(np.asarray(inputs["c_ctx"]).reshape(1, D)),
        "ml_gate_bias": f(np.asarray(inputs["ml_gate_bias"]).reshape(DEPTH, 16)),
        "hg_norm": f(np.asarray(inputs["hg_norm"]).reshape(DEPTH, 512)),
        "ml_norm": f(np.asarray(inputs["ml_norm"]).reshape(DEPTH, 512)),
    }
    for k in ("w_ada", "b_ada", "w_in", "ml_conv_w", "ml_conv_b", "hg_lower_bound", "w_out", "ln_g", "ln_b",
              "ffn_w_gate_up", "ffn_w_down", "router_w", "router_b", "moe_w_gate_up", "moe_w_down"):
        m[k] = f(inputs[k])
    return m


def kernel(**inputs):
    x = np.asarray(inputs["x"])
    B, SEQ, _ = x.shape
    if SEQ not in _CACHE:
        _CACHE[SEQ] = build_program(SEQ)
    nc = _CACHE[SEQ]
    in_maps = [_core_inputs(inputs, b) for b in range(B)]
    res = run_bass_kernel_spmd(nc, in_maps, core_ids=list(range(B)))
    return np.stack([np.asarray(r["out"], dtype=np.float32) for r in res.results], axis=0)
```

```python
import math
import os
import threading
from contextlib import ExitStack
import numpy as np
import concourse.bass as bass
import concourse.mybir as mybir
from concourse.bass_utils import run_bass_kernel_spmd

F32 = mybir.dt.float32
BF16 = mybir.dt.bfloat16
AF = mybir.ActivationFunctionType
ALU = mybir.AluOpType
AX = mybir.AxisListType

D = 1024
KC = 8
CTX = 256
PROJ = 4112
D_FF = 2816
NE = 8
D_EXP = 1408
DEPTH = 2
ALPHA = (2 * DEPTH) ** 0.25
LN_EPS = 1e-5
RMS_EPS = 1e-6


class T:
    __slots__ = ("name", "ap", "w", "rs", "dsem", "dcnt", "dload", "dwaited", "dbase")

    def __init__(self, name, ap):
        self.name = name
        self.ap = ap
        self.w = None
        self.rs = {}
        self.dsem = None
        self.dcnt = 0
        self.dload = 0
        self.dwaited = {}
        self.dbase = 0

    def __getitem__(self, k):
        return self.ap[k]


class E:
    def __init__(self, name, eng, sem):
        self.name = name
        self.e = eng
        self.sem = sem
        self.count = 0
        self.waited = {}


class Ctx:
    NO_SELF_SYNC = tuple(os.environ.get('MK_NOSELF', '').split(','))

    def __init__(self, nc, stack):
        self.nc = nc
        self.gstack = stack
        self.stack = stack
        self.engs = {}
        for name, eng in (("pe", nc.tensor), ("act", nc.scalar), ("dve", nc.vector),
                          ("pool", nc.gpsimd), ("sp", nc.sync)):
            sem = stack.enter_context(nc.semaphore("s_" + name))
            self.engs[name] = E(name, eng, sem)
        self.live = []
        self.uid = 0
        self.free_dsems = {"sw": [], "hw": []}
        self.lane_hook = None

    def sb(self, name, shape, dt):
        self.uid += 1
        t = self.stack.enter_context(self.nc.sbuf_tensor(f"{name}_{self.uid}", list(shape), dt))
        tt = T(name, t)
        self.live.append(tt)
        return tt

    def ps(self, name, shape, dt=F32):
        t = self.stack.enter_context(self.nc.psum_tensor(name, list(shape), dt))
        tt = T(name, t)
        self.live.append(tt)
        return tt

    def _dsem(self, t, q):
        kind = "sw" if q == "pool" else "hw"
        if t.dsem is None:
            if self.free_dsems[kind]:
                t.dsem = self.free_dsems[kind].pop()
            else:
                self.uid += 1
                t.dsem = [self.gstack.enter_context(self.nc.semaphore(f"d{self.uid}")), 0, kind]
            t.dbase = t.dsem[1]
        assert t.dsem[2] == kind, (t.name, kind)
        return t.dsem[0]

    def _need(self, en, rd, wr):
        Eo = self.engs[en]
        deps = {}

        def add(dep):
            if dep is None:
                return
            e2, c2 = dep
            if e2 == en and (en == "pe" or en in self.NO_SELF_SYNC):
                return
            if deps.get(e2, 0) < c2:
                deps[e2] = c2

        dwaits = []
        for t in rd:
            add(t.w)
            if t.dload > t.dwaited.get(en, 0):
                dwaits.append((t, t.dload))
        for t in wr:
            add(t.w)
            for e2, c2 in t.rs.items():
                add((e2, c2))
            if t.dcnt > t.dwaited.get(en, 0):
                dwaits.append((t, t.dcnt))
        for e2, c2 in deps.items():
            if Eo.waited.get(e2, 0) < c2:
                Eo.e.wait_ge(self.engs[e2].sem, c2)
                Eo.waited[e2] = c2
        for t, c in dwaits:
            if t.dwaited.get(en, 0) < c:
                Eo.e.wait_ge(t.dsem[0], t.dbase + 16 * c)
                t.dwaited[en] = c

    def op(self, en, fn, rd=(), wr=()):
        Eo = self.engs[en]
        self._need(en, rd, wr)
        ins = fn(Eo.e)
        Eo.count += 1
        ins.then_inc(Eo.sem, 1)
        c = Eo.count
        for t in rd:
            if t.rs.get(en, 0) < c:
                t.rs[en] = c
        for t in wr:
            t.w = (en, c)
            t.rs = {}
        if self.lane_hook is not None:
            self.lane_hook()
        return ins

    def dma(self, q, out, in_, rd=None, wr=None):
        Eo = self.engs[q]
        rdl = [rd] if rd is not None else []
        wrl = [wr] if wr is not None else []
        self._need(q, rdl, wrl)
        ins = Eo.e.dma_start(out=out, in_=in_)
        t = wr if wr is not None else rd
        sem = self._dsem(t, q)
        t.dcnt += 1
        ins.then_inc(sem, 16)
        if wr is not None:
            wr.dload = wr.dcnt
            wr.w = None
            wr.rs = {}
        return ins

    def barrier(self):
        names = list(self.engs)
        for en in names:
            Eo = self.engs[en]
            for e2 in names:
                if e2 == en:
                    continue
                c2 = self.engs[e2].count
                if c2 > Eo.waited.get(e2, 0):
                    Eo.e.wait_ge(self.engs[e2].sem, c2)
                    Eo.waited[e2] = c2
            for t in self.live:
                if t.dcnt > t.dwaited.get(en, 0):
                    Eo.e.wait_ge(t.dsem[0], t.dbase + 16 * t.dcnt)
                    t.dwaited[en] = t.dcnt

    def phase(self):
        return _Phase(self)


class _Lanes:
    def __init__(self, n):
        self.alive = [True] * n
        self.cur = 0
        self.cv = threading.Condition()
        self.err = None

    def _advance(self):
        n = len(self.alive)
        for d in range(1, n + 1):
            j = (self.cur + d) % n
            if self.alive[j]:
                self.cur = j
                return
        self.cur = -1

    def switch(self):
        with self.cv:
            me = self.cur
            self._advance()
            if self.cur == me:
                return
            self.cv.notify_all()
            while self.cur != me:
                self.cv.wait()


def run_lanes(K, fns):
    if len(fns) == 0:
        return
    if len(fns) == 1:
        fns[0]()
        return
    L = _Lanes(len(fns))

    def worker(i, fn):
        with L.cv:
            while L.cur != i:
                L.cv.wait()
        try:
            fn()
        except BaseException as e:
            L.err = e
        with L.cv:
            L.alive[i] = False
            L._advance()
            L.cv.notify_all()

    K.lane_hook = L.switch
    ths = [threading.Thread(target=worker, args=(i, f)) for i, f in enumerate(fns)]
    for t in ths:
        t.start()
    for t in ths:
        t.join()
    K.lane_hook = None
    if L.err is not None:
        raise L.err


class _Phase:
    def __init__(self, K):
        self.K = K

    def __enter__(self):
        self.prev = self.K.stack
        self.mark = len(self.K.live)
        self.st = ExitStack()
        self.st.__enter__()
        self.K.stack = self.st
        return self

    def __exit__(self, *a):
        self.K.barrier()
        for t in self.K.live[self.mark:]:
            if t.dsem is not None:
                t.dsem[1] = t.dbase + 16 * t.dcnt
                self.K.free_dsems[t.dsem[2]].append(t.dsem)
                t.dsem = None
        del self.K.live[self.mark:]
        self.K.stack = self.prev
        self.st.__exit__(*a)
        return False


def mm(K, out, lhsT, rhs, rd, wr, start=True, stop=True, tp=None):
    if tp is None:
        return K.op("pe", lambda e: e.matmul(out, lhsT=lhsT, rhs=rhs, start=start, stop=stop), rd=rd, wr=wr)
    return K.op("pe", lambda e: e.matmul(out, lhsT=lhsT, rhs=rhs, start=start, stop=stop, tile_position=tp),
                rd=rd, wr=wr)


def act(K, out, in_, func, rd, wr, bias=None, scale=None):
    kw = {}
    if bias is not None:
        kw["bias"] = bias
    if scale is not None:
        kw["scale"] = scale
    return K.op("act", lambda e: e.activation(out=out, in_=in_, func=func, **kw), rd=rd, wr=wr)


def tt(K, en, out, in0, in1, op, rd, wr):
    return K.op(en, lambda e: e.tensor_tensor(out=out, in0=in0, in1=in1, op=op), rd=rd, wr=wr)


def ts(K, en, out, in0, s1, s2, op0, op1, rd, wr):
    if op1 is None:
        return K.op(en, lambda e: e.tensor_scalar(out=out, in0=in0, scalar1=s1, scalar2=None, op0=op0), rd=rd, wr=wr)
    return K.op(en, lambda e: e.tensor_scalar(out=out, in0=in0, scalar1=s1, scalar2=s2, op0=op0, op1=op1),
                rd=rd, wr=wr)


def stt(K, out, in0, scalar, in1, op0, op1, rd, wr):
    return K.op("dve", lambda e: e.scalar_tensor_tensor(out=out, in0=in0, scalar=scalar, in1=in1, op0=op0, op1=op1),
                rd=rd, wr=wr)


def cp(K, en, out, in_, rd, wr):
    if en == "act":
        return K.op("act", lambda e: e.copy(out=out, in_=in_), rd=rd, wr=wr)
    return K.op(en, lambda e: e.tensor_copy(out=out, in_=in_), rd=rd, wr=wr)


import os
STOP = int(os.environ.get("MK_STOP", "0"))


class _Stop(Exception):
    pass


def stop_at(n):
    if STOP == n:
        raise _Stop()


def build_program(SEQ):
    nc = bass.Bass("TRN2", target_bir_lowering=False)
    try:
        _build_body(nc, SEQ)
    except _Stop:
        pass
    return nc


def _build_body(nc, SEQ):
    NLT = SEQ // 128
    NCT = CTX // 128
    NG = NLT + NCT

    def din(name, shape):
        return nc.dram_tensor(name, list(shape), F32, kind="ExternalInput").ap()

    x_d = din("x", [SEQ, D])
    c_d = din("c", [1, D])
    ctx_d = din("ctx", [CTX, D])
    cctx_d = din("c_ctx", [1, D])
    w_ada = din("w_ada", [DEPTH, D, 6 * D])
    b_ada = din("b_ada", [DEPTH, 6 * D])
    w_in = din("w_in", [DEPTH, D, PROJ])
    conv_w = din("ml_conv_w", [DEPTH, 3, 512])
    conv_b = din("ml_conv_b", [DEPTH, 512])
    hlb = din("hg_lower_bound", [DEPTH, 2, 512])
    mgb = din("ml_gate_bias", [DEPTH, 16])
    hg_norm = din("hg_norm", [DEPTH, 512])
    ml_norm = din("ml_norm", [DEPTH, 512])
    w_out = din("w_out", [DEPTH, D, D])
    ln_g = din("ln_g", [DEPTH, 2, D])
    ln_b = din("ln_b", [DEPTH, 2, D])
    ffn_gu = din("ffn_w_gate_up", [1, D, 2 * D_FF])
    ffn_dn = din("ffn_w_down", [1, D_FF, D])
    router_w = din("router_w", [1, D, NE])
    router_b = din("router_b", [1, NE])
    moe_gu = din("moe_w_gate_up", [1, NE, D, 2 * D_EXP])
    moe_dn = din("moe_w_down", [1, NE, D_EXP, D])
    out_d = nc.dram_tensor("out", [SEQ, D], F32, kind="ExternalOutput").ap()

    X1 = nc.dram_tensor("X1", [NG * 128, D], F32, kind="Internal").ap()
    X2 = nc.dram_tensor("X2", [NG * 128, D], F32, kind="Internal").ap()
    OB = nc.dram_tensor("OB", [NG * 128, D], F32, kind="Internal").ap()

    with ExitStack() as gst:
        K = Ctx(nc, gst)
        identb = K.sb("identb", [128, 128], BF16)
        revb = K.sb("revb", [128, 128], BF16)
        identf = K.sb("identf", [128, 128], F32)
        revf = K.sb("revf", [128, 128], F32)
        trif = K.sb("trif", [128, 128], F32)
        onesf = K.sb("onesf", [128, 128], F32)
        trib = K.sb("trib", [128, 128], BF16)
        onesb = K.sb("onesb", [128, 128], BF16)
        mask4 = K.sb("mask4", [128, 4, 128], F32)
        rmask = K.sb("rmask", [128, 512], F32)
        hmask = K.sb("hmask", [128, 2], F32)
        smT = K.sb("smT", [128, 256], F32)
        condT = K.sb("condT", [128, 8, 2], BF16)
        condR = K.sb("condR", [128, 8, 2, 128], BF16)
        lbT = K.sb("lbT", [128, 2, 2, 4], F32)
        omlT = K.sb("omlT", [128, 2, 2, 4], F32)
        PB = [K.ps(f"bank{i}", [128, 512], F32) for i in range(8)]

        def mk_sel(t, pattern, base, cm, cmp, fill_in, fill):
            K.op("pool", lambda e: e.memset(t[:], fill_in), wr=[t])
            K.op("pool", lambda e: e.affine_select(out=t[:], in_=t[:], pattern=pattern, compare_op=cmp,
                                                   fill=fill, base=base, channel_multiplier=cm), rd=[t], wr=[t])

        mk_sel(identb, [[-1, 128]], 0, 1, ALU.not_equal, 0.0, 1.0)
        mk_sel(identf, [[-1, 128]], 0, 1, ALU.not_equal, 0.0, 1.0)
        mk_sel(revb, [[1, 128]], -127, 1, ALU.not_equal, 0.0, 1.0)
        mk_sel(revf, [[1, 128]], -127, 1, ALU.not_equal, 0.0, 1.0)
        mk_sel(trif, [[1, 128]], 0, -1, ALU.is_ge, 1.0, 0.0)
        mk_sel(mask4, [[0, 4], [1, 128]], 0, -1, ALU.is_ge, 1.0, 0.0)
        K.op("pool", lambda e: e.memset(onesf[:], 1.0), wr=[onesf])
        K.op("pool", lambda e: e.memset(onesb[:], 1.0), wr=[onesb])
        cp(K, "pool", trib[:], trif[:], rd=[trif], wr=[trib])
        K.op("pool", lambda e: e.memset(rmask[:], 1.0), wr=[rmask])
        rmv = rmask[:].rearrange("p (s j) -> p s j", j=128)
        K.op("pool", lambda e: e.memset(rmv[:, :, 0:1], 0.0), wr=[rmask])
        K.op("pool", lambda e: e.memset(hmask[:], 0.0), wr=[hmask])
        K.op("pool", lambda e: e.memset(hmask[0:64, 0:1], 1.0), wr=[hmask])
        K.op("pool", lambda e: e.memset(hmask[64:128, 1:2], 1.0), wr=[hmask])

        rows = []
        def add_rows(ap, n):
            r0 = sum(r for _, r in rows)
            rows.append((ap, n))
            return r0
        R_BADA = [add_rows(b_ada[l:l + 1, :].rearrange("o (r p) -> (o r) p", p=128), 48) for l in range(DEPTH)]
        R_C = add_rows(c_d.rearrange("o (r p) -> (o r) p", p=128), 8)
        R_CC = add_rows(cctx_d.rearrange("o (r p) -> (o r) p", p=128), 8)
        R_HLB = add_rows(hlb.rearrange("l d (r p) -> (l d r) p", p=128), 16)
        R_CW = add_rows(conv_w.rearrange("l t (r p) -> (l t r) p", p=128), 24)
        R_CB = add_rows(conv_b.rearrange("l (r p) -> (l r) p", p=128), 8)
        NR = sum(r for _, r in rows)
        assert NR <= 256
        with K.phase():
            stg = [K.sb("stg0", [128, 128], F32), K.sb("stg1", [128, 128], F32)]
            for s_ in stg:
                K.op("pool", lambda e: e.memset(s_[:], 0.0), wr=[s_])
            r0 = 0
            for ap, n in rows:
                done = 0
                while done < n:
                    g = (r0 + done) // 128
                    lo = (r0 + done) % 128
                    m = min(n - done, 128 - lo)
                    K.dma("sp", stg[g][lo:lo + m, :], ap[done:done + m, :], wr=stg[g])
                    done += m
                r0 += n
            for g in range(2):
                K.op("pe", lambda e: e.transpose(PB[g][:, 0:128], stg[g][:], identf[:]), rd=[stg[g], identf], wr=[PB[g]])
                cp(K, "dve", smT[:, 128 * g:128 * g + 128], PB[g][:, 0:128], rd=[PB[g]], wr=[smT])
            act(K, condT[:, :, 0], smT[:, R_C:R_C + 8], AF.Silu, rd=[smT], wr=[condT])
            act(K, condT[:, :, 1], smT[:, R_CC:R_CC + 8], AF.Silu, rd=[smT], wr=[condT])
            cp(K, "dve", condR[:].rearrange("p k s m -> p (k s) m"),
               condT[:].rearrange("p k s -> p (k s)").unsqueeze(2).to_broadcast([128, 16, 128]), rd=[condT], wr=[condR])
            hv = smT[:, R_HLB:R_HLB + 16].rearrange("p (l d r) -> p l d r", l=2, d=2)
            K.op("pool", lambda e: e.memset(lbT[:], 0.0), wr=[lbT])
            tt(K, "dve", lbT[:, 1], hv[:, 1], hv[:, 0], ALU.subtract, rd=[smT, lbT], wr=[lbT])
            act(K, lbT[:, 1], lbT[:, 1], AF.Sigmoid, rd=[lbT], wr=[lbT])
            ts(K, "dve", omlT[:], lbT[:], -1.0, 1.0, ALU.mult, ALU.add, rd=[lbT], wr=[omlT])

        def src_rows(l, gi):
            if l == 0:
                if gi < NCT:
                    return ctx_d[128 * gi:128 * gi + 128, :]
                return x_d[128 * (gi - NCT):128 * (gi - NCT) + 128, :]
            return X2[128 * gi:128 * gi + 128, :]

        stop_at(1)
        for l in range(DEPTH):
            last = l == DEPTH - 1
            with K.phase():
                modT = K.sb("modT", [128, 48, 2], F32)
                G = {}
                for j in (2, 5):
                    for st in range(2):
                        if last and st == 1:
                            continue
                        G[(j, st)] = K.sb(f"G{j}{st}", [128, D], F32)
                lnG = [K.sb("lng0", [128, D], F32)]
                lnB = [K.sb("lnb0", [128, D], F32)]
                normw = K.sb("normw", [128, D], F32)
                mgbB = K.sb("mgbB", [128, 16], F32)
                K.dma("sp", lnG[0][:], ln_g[l, 0:1, :].to_broadcast([128, D]), wr=lnG[0])
                K.dma("sp", lnB[0][:], ln_b[l, 0:1, :].to_broadcast([128, D]), wr=lnB[0])
                K.dma("sp", normw[:, 0:512], hg_norm[l:l + 1, :].to_broadcast([128, 512]), wr=normw)
                K.dma("sp", normw[:, 512:1024], ml_norm[l:l + 1, :].to_broadcast([128, 512]), wr=normw)
                K.dma("sp", mgbB[:], mgb[l:l + 1, :].to_broadcast([128, 16]), wr=mgbB)

                with K.phase():
                    wab = [K.sb(f"wab{i}", [128, 8, D], BF16) for i in range(2)]
                    bb = K.sb("bb", [128, D], F32)
                    wav = w_ada[l].rearrange("(k p) n -> p k n", p=128)
                    for j in range(6):
                        wb = wab[j % 2]
                        K.dma("pool", wb[:], wav[:, :, j * D:(j + 1) * D], wr=wb)
                        if j in (2, 5):
                            K.dma("sp", bb[:], b_ada[l:l + 1, j * D:(j + 1) * D].to_broadcast([128, D]), wr=bb)
                            for st in range(2):
                                if (j, st) not in G:
                                    continue
                                for hf in range(2):
                                    pz = PB[2 + hf]
                                    for kk in range(8):
                                        mm(K, pz[:, :], condR[:, kk, st, :], wb[:, kk, 512 * hf:512 * hf + 512],
                                           rd=[condR, wb], wr=[pz], start=kk == 0, stop=kk == 7)
                                    tt(K, "dve", G[(j, st)][:, 512 * hf:512 * hf + 512], pz[:, :],
                                       bb[:, 512 * hf:512 * hf + 512], ALU.add, rd=[pz, bb], wr=[G[(j, st)]])
                        else:
                            pz = PB[4]
                            for kc in range(8):
                                for kk in range(8):
                                    mm(K, pz[:, 2 * kc:2 * kc + 2], wb[:, kk, 128 * kc:128 * kc + 128], condT[:, kk, :],
                                       rd=[condT, wb], wr=[pz], start=kk == 0, stop=kk == 7)
                            tt(K, "dve", modT[:, 8 * j:8 * j + 8, :], pz[:, 0:16].rearrange("p (k s) -> p k s", s=2),
                               smT[:, R_BADA[l] + 8 * j:R_BADA[l] + 8 * j + 8].unsqueeze(2).to_broadcast([128, 8, 2]),
                               ALU.add, rd=[pz, smT], wr=[modT])
                    for j in (1, 4):
                        ts(K, "dve", modT[:, 8 * j:8 * j + 8, :], modT[:, 8 * j:8 * j + 8, :], 1.0, None, ALU.add, None,
                           rd=[modT], wr=[modT])

                stop_at(2)
                def macros(dirn):
                    ms = [(1, list(range(NCT)))]
                    for m in range(0, NLT, 4):
                        ms.append((0, [NCT + m + i for i in range(min(4, NLT - m))]))
                    if dirn == 1:
                        c0 = ms[0]
                        lat = ms[1:][::-1]
                        ms = [(c0[0], c0[1][::-1])] + [(s, t[::-1]) for s, t in lat]
                    return ms

                for dirn in (1, 0):
                    fwd = dirn == 0
                    with K.phase():
                        IJb = identb if fwd else revb
                        xt = [K.sb(f"xt{i}", [128, D], F32) for i in range(2)]
                        xb = [K.sb(f"xb{i}", [128, D], BF16) for i in range(1)]
                        hT = K.sb("hT", [128, 8, 512], BF16)
                        wblk = [K.sb(f"wblk{i}", [128, 8, 528], BF16) for i in range(2 if fwd else 3)]
                        qTh = K.sb("qTh", [128, 4, 512], BF16)
                        fbuf = K.sb("fbuf", [128, 4, 512], F32)
                        lfb = K.sb("lfb", [128, 4, 512], F32)
                        kTh = K.sb("kTh", [128, 4, 512], BF16)
                        qkT = K.sb("qkT", [128, 4, 512], BF16)
                        vhg = K.sb("vhg", [128, 4, 512], BF16)
                        vml = K.sb("vml", [128, 4, 4, 129], BF16)
                        gts_ = K.sb("gates", [128, 4, 16], F32)
                        Dall = K.sb("Dall", [128, 4, 576], F32)
                        QK = [K.sb(f"QK{i}", [128, 4, 576], BF16) for i in range(2)]
                        Asb = [K.sb(f"Asb{i}", [128, 4, 128], BF16) for i in range(2)]
                        khtm = K.sb("khtm", [128, 4, 128], BF16)
                        Sf = K.sb("Sf", [128, 4, 128], F32)
                        Sb = K.sb("Sb", [128, 4, 128], BF16)
                        tmpS = K.sb("tmpS", [128, 4, 128], F32)
                        Ssb = K.sb("Ssb", [128, 4, 128], BF16)
                        ktm = K.sb("ktm", [128, 4, 128], BF16)
                        Cf = K.sb("Cf", [128, 4, 129], F32)
                        Cb = K.sb("Cb", [128, 4, 129], BF16)
                        tmpC = K.sb("tmpC", [128, 4, 129], F32)
                        kTm = K.sb("kTm", [128, 4, 512], BF16)
                        sml = K.sb("sml", [128, 40], F32)
                        hl = K.sb("hl", [128, 2, 4], BF16)
                        lfm = K.sb("lfm", [128, 4, 4], F32)
                        obH = [K.sb(f"obH{i}", [128, 512], F32) for i in range(2)]
                        obM = [K.sb(f"obM{i}", [128, 512], F32) for i in range(2)]
                        decS = [K.sb(f"decS{i}", [128, 4, 1], F32) for i in range(2)]
                        if fwd:
                            gate = K.sb("gate", [128, 4, D], BF16)
                            wout = K.sb("wout", [128, 8, D], BF16)
                            ob = [K.sb(f"ob{i}", [128, D], F32) for i in range(2)]
                            xres = K.sb("xres", [128, D], F32)
                            sml2 = K.sb("sml2", [128, 16], F32)
                            ybf = K.sb("ybf", [128, D], BF16)
                            tmpA = K.sb("tmpA", [128, D], F32)
                            yT = K.sb("yT", [128, 8, 128], BF16)
                            lnscr = ln_scratch(K)
                            K.dma("pool", wout[:], w_out[l].rearrange("(k p) n -> p k n", p=128), wr=wout)
                        if os.environ.get("MK_VERBOSE"):
                            print("token-mix sbuf remaining", fwd, nc.sbuf_bytes_remaining)
                        for a_ in Asb:
                            K.op("pool", lambda e: e.memset(a_[:], 0.0), wr=[a_])
                        K.op("pool", lambda e: e.memset(vml[:], 1.0), wr=[vml])
                        K.op("pool", lambda e: e.memset(Sf[:], 0.0), wr=[Sf])
                        K.op("pool", lambda e: e.memset(Sb[:], 0.0), wr=[Sb])
                        K.op("pool", lambda e: e.memset(Cf[:], 0.0), wr=[Cf])
                        K.op("pool", lambda e: e.memset(Cb[:], 0.0), wr=[Cb])
                        K.op("pool", lambda e: e.memset(ktm[:], 0.0), wr=[ktm])
                        wiv = w_in[l].rearrange("(k p) n -> p k n", p=128)
                        cwo = R_CW + l * 12
                        cbo = R_CB + l * 4
                        cnt = {"xt": 0, "w": 0, "t": 0}

                        def step1_tile(st, gi, s):
                            if True:
                                xtt = xt[cnt["xt"] % 2]
                                xbt = xb[0]
                                cnt["xt"] += 1
                                K.dma("sp", xtt[:], src_rows(l, gi), wr=xtt)
                                cp(K, "act", xbt[:], xtt[:], rd=[xtt], wr=[xbt])
                                for hb in range(2):
                                    pt = PB[hb]
                                    for kq in range(4):
                                        k = 4 * hb + kq
                                        mm(K, pt[:, 128 * kq:128 * kq + 128], xbt[:, 128 * k:128 * k + 128], IJb[:],
                                           rd=[xbt, IJb], wr=[pt])
                                    for kq in range(4):
                                        k = 4 * hb + kq
                                        sc = modT[:, 8 + k, st:st + 1]
                                        sh = modT[:, k, st:st + 1]
                                        if kq % 2 == 0:
                                            act(K, hT[:, k, 128 * s:128 * s + 128], pt[:, 128 * kq:128 * kq + 128],
                                                AF.Identity, rd=[pt, modT], wr=[hT], bias=sh, scale=sc)
                                        else:
                                            ts(K, "dve", hT[:, k, 128 * s:128 * s + 128], pt[:, 128 * kq:128 * kq + 128],
                                               sc, sh, ALU.mult, ALU.add, rd=[pt, modT], wr=[hT])

                        def load_w(c0, ncols):
                            wbk = wblk[cnt["w"] % len(wblk)]
                            cnt["w"] += 1
                            K.dma("pool", wbk[:, :, 0:ncols], wiv[:, :, c0:c0 + ncols], wr=wbk)
                            return wbk

                        def prefetch_w():
                            return [load_w(0, 512), load_w(512 * (1 + dirn), 512)]

                        mlist = macros(dirn)
                        for s_, gi_ in enumerate(mlist[0][1]):
                            step1_tile(mlist[0][0], gi_, s_)
                        pre_w = prefetch_w()
                        for mi, (st, tiles) in enumerate(mlist):
                            nsl = len(tiles)
                            NT = 128 * nsl
                            nxt = mlist[mi + 1] if mi + 1 < len(mlist) else None
                            stop_at(3)

                            def proj_fm(wbk, c, evac):
                                pz = PB[2 + (cnt["t"] % 2)]
                                cnt["t"] += 1
                                for k in range(8):
                                    mm(K, pz[:, 0:NT], wbk[:, k, 128 * c:128 * c + 128], hT[:, k, 0:NT],
                                       rd=[wbk, hT], wr=[pz], start=k == 0, stop=k == 7)
                                evac(pz, c)

                            def proj_tm(wbk, s, ncols, evac):
                                pz = PB[2 + (cnt["t"] % 2)]
                                cnt["t"] += 1
                                for k in range(8):
                                    mm(K, pz[:, 0:ncols], hT[:, k, 128 * s:128 * s + 128], wbk[:, k, 0:ncols],
                                       rd=[wbk, hT], wr=[pz], start=k == 0, stop=k == 7)
                                evac(pz, s)

                            wbk = pre_w[0]
                            for c in range(4):
                                proj_fm(wbk, c, lambda pz, c: act(K, qTh[:, c, 0:NT], pz[:, 0:NT], AF.Silu, rd=[pz], wr=[qTh]))
                            wbk = pre_w[1]
                            for c in range(4):
                                def ev_f(pz, c):
                                    act(K, fbuf[:, c, 0:NT], pz[:, 0:NT], AF.Sigmoid, rd=[pz], wr=[fbuf])
                                    ts(K, "dve", fbuf[:, c, 0:NT], fbuf[:, c, 0:NT], omlT[:, l, dirn, c:c + 1],
                                       lbT[:, l, dirn, c:c + 1], ALU.mult, ALU.add, rd=[fbuf, omlT, lbT], wr=[fbuf])
                                proj_fm(wbk, c, ev_f)
                            act(K, lfb[:, :, 0:NT], fbuf[:, :, 0:NT], AF.Ln, rd=[fbuf], wr=[lfb])
                            act(K, kTh[:, :, 0:NT], fbuf[:, :, 0:NT], AF.Identity, rd=[fbuf], wr=[kTh], scale=-1.0, bias=1.0)
                            for c in range(4):
                                K.op("dve", lambda e: e.tensor_tensor_scan(
                                    out=fbuf[:, c, 0:NT], data0=rmask[:, 0:NT], data1=lfb[:, c, 0:NT], initial=0.0,
                                    op0=ALU.mult, op1=ALU.add), rd=[rmask, lfb], wr=[fbuf])
                            wbk = load_w(1536, 512)
                            for s in range(nsl):
                                proj_tm(wbk, s, 512, lambda pz, s: cp(K, "act", vhg[:, s, :], pz[:, :], rd=[pz], wr=[vhg]))
                            if fwd:
                                wbk = load_w(2048, 512)
                                for s in range(nsl):
                                    proj_tm(wbk, s, 512, lambda pz, s: act(K, gate[:, s, 0:512], pz[:, :], AF.Silu,
                                                                           rd=[pz], wr=[gate]))
                            wbk = load_w(2560, 512)
                            rw = 64 if st == 0 else NT
                            wp, wn = (0, 2) if fwd else (2, 0)
                            for c in range(4):
                                def ev_c(pz, c):
                                    cw = lambda tap: smT[:, cwo + 4 * tap + c:cwo + 4 * tap + c + 1]
                                    cv = lfb[:, c, 0:NT]
                                    ts(K, "dve", cv, pz[:, 0:NT], cw(1), smT[:, cbo + c:cbo + c + 1], ALU.mult, ALU.add,
                                       rd=[pz, smT], wr=[lfb])
                                    cv3 = cv.rearrange("p (r j) -> p r j", j=rw)
                                    pz3 = pz[:, 0:NT].rearrange("p (r j) -> p r j", j=rw)
                                    stt(K, cv3[:, :, 1:rw], pz3[:, :, 0:rw - 1], cw(wp), cv3[:, :, 1:rw], ALU.mult, ALU.add,
                                        rd=[pz, smT, lfb], wr=[lfb])
                                    stt(K, cv3[:, :, 0:rw - 1], pz3[:, :, 1:rw], cw(wn), cv3[:, :, 0:rw - 1], ALU.mult, ALU.add,
                                        rd=[pz, smT, lfb], wr=[lfb])
                                proj_fm(wbk, c, ev_c)
                            act(K, qkT[:, :, 0:NT], lfb[:, :, 0:NT], AF.Silu, rd=[lfb], wr=[qkT])
                            for h in range(4):
                                if h % 2:
                                    act(K, kTm[:, h, 0:NT], qkT[:, 2 + h // 2, 0:NT], AF.Copy, rd=[qkT, hmask], wr=[kTm],
                                        scale=hmask[:, h % 2:h % 2 + 1])
                                else:
                                    ts(K, "dve", kTm[:, h, 0:NT], qkT[:, 2 + h // 2, 0:NT], hmask[:, h % 2:h % 2 + 1], None,
                                       ALU.mult, None, rd=[qkT, hmask], wr=[kTm])
                            wbk = load_w(3072, 512)
                            for s in range(nsl):
                                proj_tm(wbk, s, 512, lambda pz, s: cp(K, "act", vml[:, s, :, 0:128],
                                                                      pz[:, :].rearrange("p (h e) -> p h e", e=128),
                                                                      rd=[pz], wr=[vml]))
                            if fwd:
                                wbk = load_w(3584, 528)
                                for s in range(nsl):
                                    def ev_g(pz, s):
                                        tt(K, "dve", gts_[:, s, :], pz[:, 0:16], mgbB[:], ALU.add, rd=[pz, mgbB], wr=[gts_])
                                    proj_tm(wbk, s, 16, ev_g)
                                for s in range(nsl):
                                    pz = PB[2 + (cnt["t"] % 2)]
                                    cnt["t"] += 1
                                    for k in range(8):
                                        mm(K, pz[:, 0:512], hT[:, k, 128 * s:128 * s + 128], wbk[:, k, 16:528],
                                           rd=[wbk, hT], wr=[pz], start=k == 0, stop=k == 7)
                                    act(K, gate[:, s, 512:1024], pz[:, :], AF.Sigmoid, rd=[pz], wr=[gate])
                            else:
                                wbk = load_w(3584, 16)
                                for s in range(nsl):
                                    def ev_g(pz, s):
                                        tt(K, "dve", gts_[:, s, :], pz[:, 0:16], mgbB[:], ALU.add, rd=[pz, mgbB], wr=[gts_])
                                    proj_tm(wbk, s, 16, ev_g)

                            act(K, lfm[:, 0:nsl, :], gts_[:, 0:nsl, 8 * dirn + 4:8 * dirn + 8], AF.Sigmoid, rd=[gts_], wr=[lfm])
                            act(K, lfm[:, 0:nsl, :], lfm[:, 0:nsl, :], AF.Ln, rd=[lfm], wr=[lfm])
                            stop_at(4)
                            need_out = not (last and st == 1)
                            offs = {0: 0, 1: 128, 2: 224, 3: 288}

                            def lane_hgA(s):
                                tsl = slice(128 * s, 128 * s + 128)
                                qk = QK[s % 2]
                                g_ = fbuf[:, :, tsl]
                                cp(K, "act", Dall[:, :, 0:128], g_, rd=[fbuf], wr=[Dall])
                                for a in (1, 2, 3):
                                    n = 128 - 32 * a
                                    tt(K, "dve", Dall[:, :, offs[a]:offs[a] + n], g_[:, :, 32 * a:128],
                                       g_[:, :, 32 * a - 1:32 * a].to_broadcast([128, 4, n]), ALU.subtract, rd=[fbuf], wr=[Dall])
                                    tt(K, "dve", Dall[:, :, 320 + 32 * a:352 + 32 * a],
                                       g_[:, :, 32 * a - 1:32 * a].to_broadcast([128, 4, 32]), g_[:, :, 32 * a:32 * a + 32],
                                       ALU.subtract, rd=[fbuf], wr=[Dall])
                                act(K, Dall[:, :, 320:352], g_[:, :, 0:32], AF.Copy, rd=[fbuf], wr=[Dall], scale=-1.0)
                                tt(K, "dve", Dall[:, :, 448:576], g_[:, :, 127:128].to_broadcast([128, 4, 128]), g_,
                                   ALU.subtract, rd=[fbuf], wr=[Dall])
                                act(K, Dall[:], Dall[:], AF.Exp, rd=[Dall], wr=[Dall])
                                for a in range(4):
                                    n = 128 - 32 * a
                                    tt(K, "dve", qk[:, :, offs[a]:offs[a] + n],
                                       qTh[:, :, 128 * s + 32 * a:128 * s + 128], Dall[:, :, offs[a]:offs[a] + n], ALU.mult,
                                       rd=[qTh, Dall], wr=[qk])
                                tt(K, "dve", qk[:, :, 320:448], kTh[:, :, tsl], Dall[:, :, 320:448], ALU.mult, rd=[kTh, Dall], wr=[qk])
                                tt(K, "dve", qk[:, :, 448:576], kTh[:, :, tsl], Dall[:, :, 448:576], ALU.mult, rd=[kTh, Dall], wr=[qk])
                                cp(K, "dve", decS[s % 2][:], Dall[:, :, 127:128], rd=[Dall], wr=[decS[s % 2]])

                            def lane_hgB(s, gi):
                                tsl = slice(128 * s, 128 * s + 128)
                                qk = QK[s % 2]
                                asb = Asb[s % 2]
                                obh = obH[s % 2]
                                pa = PB[4]
                                for h in range(4):
                                    for a in range(4):
                                        n = 128 - 32 * a
                                        mm(K, pa[32 * a:32 * a + 32, 128 * h + 32 * a:128 * h + 128],
                                           qk[:, h, 320 + 32 * a:352 + 32 * a], qk[:, h, offs[a]:offs[a] + n],
                                           rd=[qk], wr=[pa], tp=(0, 32 * a))
                                pa3 = pa[:, :].rearrange("p (h t) -> p h t", t=128)
                                for a in range(4):
                                    tt(K, "dve", asb[32 * a:32 * a + 32, :, 32 * a:128], pa3[32 * a:32 * a + 32, :, 32 * a:128],
                                       mask4[32 * a:32 * a + 32, :, 32 * a:128], ALU.mult, rd=[pa, mask4], wr=[asb])
                                pk = PB[5]
                                for h in range(4):
                                    mm(K, pk[:, 128 * h:128 * h + 128], qk[:, h, 448:576], identb[:], rd=[qk, identb], wr=[pk])
                                cp(K, "act", khtm[:].rearrange("p h d -> p (h d)"), pk[:, :], rd=[pk], wr=[khtm])
                                po = PB[6]
                                if need_out:
                                    first = True
                                    if fwd:
                                        obt = ob[s % 2]
                                        mm(K, po[:, :], revf[:], obt[:, 0:512], rd=[revf, obt], wr=[po], start=True, stop=False)
                                        first = False
                                    for h in range(4):
                                        hs = slice(128 * h, 128 * h + 128)
                                        mm(K, po[:, hs], asb[:, h, :], vhg[:, s, hs], rd=[asb, vhg], wr=[po],
                                           start=first, stop=False)
                                        mm(K, po[:, hs], qk[:, h, 0:128], Sb[:, h, :], rd=[qk, Sb], wr=[po],
                                           start=False, stop=(h == 3) if fwd else True)
                                pu = PB[4]
                                for h in range(4):
                                    hs = slice(128 * h, 128 * h + 128)
                                    mm(K, pu[:, hs], khtm[:, h, :], vhg[:, s, hs], rd=[khtm, vhg], wr=[pu])
                                tt(K, "dve", tmpS[:], Sf[:], decS[s % 2][:].to_broadcast([128, 4, 128]), ALU.mult,
                                   rd=[Sf, decS[s % 2]], wr=[tmpS])
                                tt(K, "dve", Sf[:], tmpS[:], pu[:, :].rearrange("p (h e) -> p h e", e=128), ALU.add,
                                   rd=[tmpS, pu], wr=[Sf])
                                cp(K, "act", Sb[:], Sf[:], rd=[Sf], wr=[Sb])
                                if need_out:
                                    cp(K, "act", obh[:], po[:, :], rd=[po], wr=[obh])
                                    if not fwd:
                                        K.dma("sp", OB[128 * gi:128 * gi + 128, 0:512], obh[:], rd=obh)

                            def lane_ml(s, gi):
                                tsl = slice(128 * s, 128 * s + 128)
                                obm = obM[s % 2]
                                ic = gts_[:, s, 8 * dirn:8 * dirn + 4]
                                fg = gts_[:, s, 8 * dirn + 4:8 * dirn + 8]
                                lf_ = lfm[:, s, :]
                                pb = PB[3]
                                cp(K, "dve", hl[:, 0, :], lf_, rd=[lfm], wr=[hl])
                                tt(K, "dve", hl[:, 1, :], lf_, hl[:, 0, :], ALU.subtract, rd=[lfm, hl], wr=[hl])
                                for i2 in range(2):
                                    mm(K, pb[:, 0:4], trib[:], hl[:, i2, :], rd=[trib, hl], wr=[pb], start=i2 == 0, stop=i2 == 1)
                                for i2 in range(2):
                                    mm(K, pb[:, 4:8], onesb[:], hl[:, i2, :], rd=[onesb, hl], wr=[pb], start=i2 == 0, stop=i2 == 1)
                                tt(K, "dve", sml[:, 4:8], ic, pb[:, 0:4], ALU.subtract, rd=[gts_, pb], wr=[sml])
                                act(K, sml[:, 4:8], sml[:, 4:8], AF.Exp, rd=[sml], wr=[sml])
                                act(K, sml[:, 8:12], pb[:, 0:4], AF.Exp, rd=[pb], wr=[sml], scale=-1.0, bias=math.log(8.0))
                                act(K, sml[:, 12:16], pb[:, 4:8], AF.Exp, rd=[pb], wr=[sml])
                                psS = PB[7]
                                for h in range(4):
                                    g = h // 2
                                    mm(K, psS[:, 128 * h:128 * h + 128], kTm[:, h, tsl], qkT[:, g, tsl], rd=[kTm, qkT], wr=[psS])
                                for h in range(4):
                                    stt(K, Ssb[:, h, :], psS[:, 128 * h:128 * h + 128], sml[:, 4 + h:5 + h], mask4[:, h, :],
                                        ALU.mult, ALU.mult, rd=[psS, sml, mask4], wr=[Ssb])
                                for g in range(2):
                                    mm(K, pb[:, 16 + 128 * g:144 + 128 * g], qkT[:, 2 + g, tsl], identb[:], rd=[qkT, identb], wr=[pb])
                                pbk = pb[:, 16:272].rearrange("p (g h d) -> p g h d", g=2, h=2)
                                ktv = ktm[:].rearrange("p (g h) (x d) -> p g h x d", h=2, x=2)
                                for hp in range(2):
                                    tt(K, "dve", ktv[:, :, hp, hp, :], pbk[:, :, hp, :],
                                       sml[:, 4 + hp:8:2].unsqueeze(2).to_broadcast([128, 2, 64]), ALU.mult, rd=[pb, sml], wr=[ktm])
                                pn = PB[7]
                                pd = PB[3]
                                if need_out:
                                    for h in range(4):
                                        g = h // 2
                                        hs = slice(128 * h, 128 * h + 128)
                                        mm(K, pn[:, hs], Ssb[:, h, :], vml[:, s, h, 0:128], rd=[Ssb, vml], wr=[pn], start=True, stop=False)
                                        mm(K, pn[:, hs], qkT[:, g, tsl], Cb[:, h, 0:128], rd=[qkT, Cb], wr=[pn],
                                           start=False, stop=True)
                                        mm(K, pd[:, 8 + h:9 + h], Ssb[:, h, :], vml[:, s, h, 128:129], rd=[Ssb, vml], wr=[pd],
                                           start=True, stop=False)
                                        mm(K, pd[:, 8 + h:9 + h], qkT[:, g, tsl], Cb[:, h, 128:129],
                                           rd=[qkT, Cb], wr=[pd], start=False, stop=True)
                                    tt(K, "dve", sml[:, 16:20], pd[:, 8:12], sml[:, 8:12], ALU.max, rd=[pd, sml], wr=[sml])
                                    stt(K, sml[:, 16:20], pd[:, 8:12], -1.0, sml[:, 16:20], ALU.mult, ALU.max, rd=[pd, sml], wr=[sml])
                                    K.op("dve", lambda e: e.reciprocal(out=sml[:, 20:24], in_=sml[:, 16:20]), rd=[sml], wr=[sml])
                                    tt(K, "dve", obm[:].rearrange("p (h e) -> p h e", e=128),
                                       pn[:, :].rearrange("p (h e) -> p h e", e=128),
                                       sml[:, 20:24].unsqueeze(2).to_broadcast([128, 4, 128]), ALU.mult, rd=[pn, sml], wr=[obm])
                                    if not fwd:
                                        K.dma("sp", OB[128 * gi:128 * gi + 128, 512:1024], obm[:], rd=obm)
                                for g in range(2):
                                    pc = PB[7] if g == 0 else PB[3]
                                    for hp in range(2):
                                        h = 2 * g + hp
                                        mm(K, pc[:, 129 * hp:129 * hp + 129], ktm[:, h, :], vml[:, s, h, :], rd=[ktm, vml], wr=[pc])
                                    tt(K, "dve", tmpC[:, 2 * g:2 * g + 2, :], Cf[:, 2 * g:2 * g + 2, :],
                                       pc[:, 0:258].rearrange("p (g e) -> p g e", e=129), ALU.add, rd=[Cf, pc], wr=[tmpC])
                                tt(K, "dve", Cf[:], tmpC[:], sml[:, 12:16].unsqueeze(2).to_broadcast([128, 4, 129]), ALU.mult,
                                   rd=[tmpC, sml], wr=[Cf])
                                cp(K, "act", Cb[:], Cf[:], rd=[Cf], wr=[Cb])

                            def lane_epi(s, gi):
                                obh, obm, obt = obH[s % 2], obM[s % 2], ob[s % 2]
                                pj = PB[2]
                                mm(K, pj[:, :], revf[:], obt[:, 512:1024], rd=[revf, obt], wr=[pj])
                                tt(K, "dve", obm[:], obm[:], pj[:, :], ALU.add, rd=[obm, pj], wr=[obm])
                                tt(K, "dve", tmpA[:, 0:512], obh[:], obh[:], ALU.mult, rd=[obh], wr=[tmpA])
                                tt(K, "dve", tmpA[:, 512:1024], obm[:], obm[:], ALU.mult, rd=[obm], wr=[tmpA])
                                K.op("dve", lambda e: e.tensor_reduce(out=sml2[:, 0:8], in_=tmpA[:].rearrange("p (h e) -> p h e", e=128),
                                                                      axis=AX.X, op=ALU.add), rd=[tmpA], wr=[sml2])
                                act(K, sml2[:, 0:8], sml2[:, 0:8], AF.Ln, rd=[sml2], wr=[sml2], scale=1.0 / 128.0, bias=RMS_EPS)
                                act(K, sml2[:, 8:16], sml2[:, 0:8], AF.Exp, rd=[sml2], wr=[sml2], scale=-0.5)
                                tt(K, "dve", tmpA[:, 0:512].rearrange("p (h e) -> p h e", e=128), obh[:].rearrange("p (h e) -> p h e", e=128),
                                   sml2[:, 8:12].unsqueeze(2).to_broadcast([128, 4, 128]), ALU.mult, rd=[obh, sml2], wr=[tmpA])
                                tt(K, "dve", tmpA[:, 512:1024].rearrange("p (h e) -> p h e", e=128), obm[:].rearrange("p (h e) -> p h e", e=128),
                                   sml2[:, 12:16].unsqueeze(2).to_broadcast([128, 4, 128]), ALU.mult, rd=[obm, sml2], wr=[tmpA])
                                tt(K, "dve", tmpA[:], tmpA[:], gate[:, s, :], ALU.mult, rd=[tmpA, gate], wr=[tmpA])
                                tt(K, "dve", ybf[:], tmpA[:], normw[:], ALU.mult, rd=[tmpA, normw], wr=[ybf])
                                for hb in range(2):
                                    pt = PB[hb]
                                    for kq in range(4):
                                        k = 4 * hb + kq
                                        mm(K, pt[:, 128 * kq:128 * kq + 128], ybf[:, 128 * k:128 * k + 128], identb[:],
                                           rd=[ybf, identb], wr=[pt])
                                    cp(K, "act" if hb == 0 else "dve", yT[:, 4 * hb:4 * hb + 4, :].rearrange("p k t -> p (k t)"),
                                       pt[:, :], rd=[pt], wr=[yT])
                                K.dma("sp", xres[:], src_rows(l, gi), wr=xres)
                                for hf in range(2):
                                    pz = PB[hf]
                                    cs = slice(512 * hf, 512 * hf + 512)
                                    for k in range(8):
                                        mm(K, pz[:, :], yT[:, k, :], wout[:, k, cs], rd=[yT, wout], wr=[pz], start=k == 0, stop=k == 7)
                                    tt(K, "dve", tmpA[:, cs], pz[:, :], G[(2, st)][:, cs], ALU.mult, rd=[pz, G[(2, st)]], wr=[tmpA])
                                stt(K, tmpA[:], xres[:], ALPHA, tmpA[:], ALU.mult, ALU.add, rd=[xres, tmpA], wr=[tmpA])
                                layer_norm(K, tmpA, tmpA, lnG[0], lnB[0], lnscr)
                                K.dma("sp", X1[128 * gi:128 * gi + 128, :], tmpA[:], rd=tmpA)

                            do_epi = fwd and need_out
                            if nxt is not None:
                                pre_w = prefetch_w()
                            nnx = len(nxt[1]) if nxt is not None else 0
                            if not os.environ.get("MK_LANE1") and nxt is not None:
                                for s_, gi_ in enumerate(nxt[1]):
                                    step1_tile(nxt[0], gi_, s_)
                                nnx = 0
                            lane_hgA(0)
                            for s in range(max(nsl + (1 if do_epi else 0), nnx)):
                                fns = []
                                if s < nsl:
                                    gi = tiles[s]
                                    if do_epi:
                                        K.dma("sp", ob[s % 2][:], OB[128 * gi:128 * gi + 128, :], wr=ob[s % 2])
                                    fns.append(lambda s=s, gi=gi: lane_hgB(s, gi))
                                    if s + 1 < nsl:
                                        fns.append(lambda s=s: lane_hgA(s + 1))
                                    fns.append(lambda s=s, gi=gi: lane_ml(s, gi))
                                def lane_c(s=s):
                                    if do_epi and 1 <= s <= nsl:
                                        lane_epi(s - 1, tiles[s - 1])
                                    if s < nnx:
                                        step1_tile(nxt[0], nxt[1][s], s)
                                fns.append(lane_c)
                                run_lanes(K, fns)
                                stop_at(6)

                    stop_at(7 + (1 - dirn))
                with K.phase():
                    if not last:
                        ffn_dense(K, nc, PB, identf, modT, G, ln_g[l, 1:2, :], ln_b[l, 1:2, :], X1, X2, ffn_gu, ffn_dn, NG, NCT)
                    else:
                        ffn_moe(K, nc, PB, identf, onesf, modT, G, ln_g[l, 1:2, :], ln_b[l, 1:2, :], X1, out_d, router_w, router_b,
                                moe_gu, moe_dn, NG, NCT)
            stop_at(9 + l)
        K.barrier()
    return nc


def ln_scratch(K):
    return (K.sb("st6", [128, 2, 6], F32), K.sb("mv2", [128, 2], F32))


def layer_norm(K, src, dst, g, b, scr):
    st6, mv = scr
    for i in range(2):
        K.op("dve", lambda e: e.bn_stats(out=st6[:, i, :], in_=src[:, 512 * i:512 * i + 512]), rd=[src], wr=[st6])
    K.op("dve", lambda e: e.bn_aggr(out=mv[:], in_=st6[:].rearrange("p a b -> p (a b)")), rd=[st6], wr=[mv])
    if os.environ.get("MK_LNSQRT"):
        act(K, mv[:, 1:2], mv[:, 1:2], AF.Sqrt, rd=[mv], wr=[mv], bias=LN_EPS)
        K.op("dve", lambda e: e.reciprocal(out=mv[:, 1:2], in_=mv[:, 1:2]), rd=[mv], wr=[mv])
    else:
        act(K, mv[:, 1:2], mv[:, 1:2], AF.Ln, rd=[mv], wr=[mv], bias=LN_EPS)
        act(K, mv[:, 1:2], mv[:, 1:2], AF.Exp, rd=[mv], wr=[mv], scale=-0.5)
    ts(K, "dve", src[:], src[:], mv[:, 0:1], mv[:, 1:2], ALU.subtract, ALU.mult, rd=[src, mv], wr=[src])
    tt(K, "dve", src[:], src[:], g[:], ALU.mult, rd=[src, g], wr=[src])
    tt(K, "dve", dst[:], src[:], b[:], ALU.add, rd=[src, b], wr=[dst])


def ffn_load_h(K, PB, identf, modT, X1, gi, st, xt, hT32, hT, s):
    K.dma("sp", xt[:], X1[128 * gi:128 * gi + 128, :], wr=xt)
    for hb in range(2):
        pt = PB[hb]
        for kq in range(4):
            k = 4 * hb + kq
            K.op("pe", lambda e: e.transpose(pt[:, 128 * kq:128 * kq + 128], xt[:, 128 * k:128 * k + 128], identf[:]),
                 rd=[xt, identf], wr=[pt])
        for kq in range(4):
            k = 4 * hb + kq
            sc = modT[:, 32 + k, st:st + 1]
            sh = modT[:, 24 + k, st:st + 1]
            dst = hT32[:, k, :] if hT32 is not None else hT[:, k, 128 * s:128 * s + 128]
            if kq % 2 == 0:
                act(K, dst, pt[:, 128 * kq:128 * kq + 128], AF.Identity, rd=[pt, modT], wr=[hT32 if hT32 is not None else hT],
                    bias=sh, scale=sc)
            else:
                ts(K, "dve", dst, pt[:, 128 * kq:128 * kq + 128], sc, sh, ALU.mult, ALU.add, rd=[pt, modT],
                   wr=[hT32 if hT32 is not None else hT])
    if hT32 is not None:
        cp(K, "act", hT[:, :, 128 * s:128 * s + 128], hT32[:], rd=[hT32], wr=[hT])


def ffn_dense(K, nc, PB, identf, modT, G, lng_d, lnb_d, X1, X2, ffn_gu, ffn_dn, NG, NCT):
    NCH = D_FF // 128
    lng = K.sb("flng", [128, D], F32)
    lnb = K.sb("flnb", [128, D], F32)
    K.dma("sp", lng[:], lng_d.to_broadcast([128, D]), wr=lng)
    K.dma("sp", lnb[:], lnb_d.to_broadcast([128, D]), wr=lnb)
    xt = [K.sb(f"fxt{i}", [128, D], F32) for i in range(2)]
    xrs = [K.sb(f"fxr{i}", [128, D], F32) for i in range(2)]
    hTs = [K.sb(f"fhT{i}", [128, 8, 512], BF16) for i in range(2)]
    wgu = [K.sb(f"wgu{i}", [128, 8, 2, 256], BF16) for i in range(3)]
    hid = K.sb("hid", [128, NCH, 512], BF16)
    sg = [K.sb(f"sg{i}", [128, 512], F32) for i in range(2)]
    wdn = [K.sb(f"wdn{i}", [128, NCH, 512], BF16) for i in range(2)]
    tmpA = K.sb("ftmpA", [128, D], F32)
    xo = K.sb("fxo", [128, D], F32)
    sml = ln_scratch(K)
    guv = ffn_gu[0].rearrange("(k p) n -> p k n", p=128)
    dnv = ffn_dn[0].rearrange("(c p) n -> p c n", p=128)
    macs = [(1, list(range(NCT)))]
    for m in range(NCT, NG, 4):
        macs.append((0, list(range(m, min(m + 4, NG)))))
    cntw = 0
    for hf in range(2):
        K.dma("pool", wdn[hf][:], dnv[:, :, 512 * hf:512 * hf + 512], wr=wdn[hf])

    def load_macro(mi):
        st_, tiles_ = macs[mi]
        for s_, gi_ in enumerate(tiles_):
            ffn_load_h(K, PB, identf, modT, X1, gi_, st_, xt[s_ % 2], None, hTs[mi % 2], s_)

    load_macro(0)
    for mi, (st, tiles) in enumerate(macs):
        nsl = len(tiles)
        NT = 128 * nsl
        hT = hTs[mi % 2]
        for blk in range(NCH // 2):
            wb = wgu[cntw % 3]
            cntw += 1
            K.dma("pool", wb[:, :, 0, :], guv[:, :, 256 * blk:256 * blk + 256], wr=wb)
            K.dma("pool", wb[:, :, 1, :], guv[:, :, D_FF + 256 * blk:D_FF + 256 * blk + 256], wr=wb)
            for cc in range(2):
                c = 2 * blk + cc
                pg, pu = PB[2 + (c % 2) * 2], PB[3 + (c % 2) * 2]
                for k in range(8):
                    mm(K, pg[:, 0:NT], wb[:, k, 0, 128 * cc:128 * cc + 128], hT[:, k, 0:NT], rd=[wb, hT], wr=[pg],
                       start=k == 0, stop=k == 7)
                for k in range(8):
                    mm(K, pu[:, 0:NT], wb[:, k, 1, 128 * cc:128 * cc + 128], hT[:, k, 0:NT], rd=[wb, hT], wr=[pu],
                       start=k == 0, stop=k == 7)
                sgt = sg[c % 2]
                act(K, sgt[:, 0:NT], pg[:, 0:NT], AF.Silu, rd=[pg], wr=[sgt])
                tt(K, "dve", hid[:, c, 0:NT], sgt[:, 0:NT], pu[:, 0:NT], ALU.mult, rd=[sgt, pu], wr=[hid])
        if mi + 1 < len(macs):
            load_macro(mi + 1)
        for s, gi in enumerate(tiles):
            for hf in range(2):
                pz = PB[6 + hf]
                cs = slice(512 * hf, 512 * hf + 512)
                for c in range(NCH):
                    mm(K, pz[:, :], hid[:, c, 128 * s:128 * s + 128], wdn[hf][:, c, :], rd=[hid, wdn[hf]], wr=[pz],
                       start=c == 0, stop=c == NCH - 1)
                tt(K, "dve", tmpA[:, cs], pz[:, :], G[(5, st)][:, cs], ALU.mult, rd=[pz, G[(5, st)]], wr=[tmpA])
            xr = xrs[s % 2]
            K.dma("sp", xr[:], X1[128 * gi:128 * gi + 128, :], wr=xr)
            stt(K, tmpA[:], xr[:], ALPHA, tmpA[:], ALU.mult, ALU.add, rd=[xr, tmpA], wr=[tmpA])
            layer_norm(K, tmpA, xo, lng, lnb, sml)
            K.dma("sp", X2[128 * gi:128 * gi + 128, :], xo[:], rd=xo)


def ffn_moe(K, nc, PB, identf, onesf, modT, G, lng_d, lnb_d, X1, out_d, router_w, router_b, moe_gu, moe_dn, NG, NCT):
    NCH = D_EXP // 128
    lng = K.sb("mlng", [128, D], F32)
    lnb = K.sb("mlnb", [128, D], F32)
    K.dma("sp", lng[:], lng_d.to_broadcast([128, D]), wr=lng)
    K.dma("sp", lnb[:], lnb_d.to_broadcast([128, D]), wr=lnb)
    NLT = NG - NCT
    MT = 8 if NLT % 8 == 0 else 4
    xt = [K.sb(f"mxt{i}", [128, D], F32) for i in range(2)]
    hT = K.sb("mhT", [128, 8, 128 * MT], BF16)
    hT32 = K.sb("mhT32", [128, 8, 128], F32)
    rw = K.sb("rw", [128, 8, NE], F32)
    rwh = K.sb("rwh", [128, 8, NE], BF16)
    rwl = K.sb("rwl", [128, 8, NE], BF16)
    hlo = K.sb("hlo", [128, 8, 128], BF16)
    rbB = K.sb("rbB", [128, NE], F32)
    comb = K.sb("comb", [128, MT, NE], F32)
    lg = K.sb("lg", [128, 32], F32)
    wgu = [K.sb(f"mwgu{i}", [128, 8, 2, 128], BF16) for i in range(3)]
    hid = K.sb("mhid", [128, NCH, 128 * MT], BF16)
    sg = [K.sb(f"msg{i}", [128, 512], F32) for i in range(2)]
    wdn = [K.sb(f"mwdn{i}", [128, NCH, D], BF16) for i in range(2)]
    acc = K.sb("macc", [128, MT, D], F32)
    tmpA = K.sb("mtmpA", [128, D], F32)
    xo = K.sb("mxo", [128, D], F32)
    lnscr = ln_scratch(K)
    K.dma("sp", rw[:], router_w[0].rearrange("(k p) e -> p k e", p=128), wr=rw)
    K.dma("sp", rbB[:], router_b[0:1, :].to_broadcast([128, NE]), wr=rbB)
    cp(K, "dve", rwh[:], rw[:], rd=[rw], wr=[rwh])
    tt(K, "dve", rwl[:], rw[:], rwh[:], ALU.subtract, rd=[rw, rwh], wr=[rwl])
    cntw = 0
    for m0 in range(NCT, NG, MT):
        tiles = list(range(m0, m0 + MT))
        NT = 128 * MT
        NH = NT // 512
        for s, gi in enumerate(tiles):
            ffn_load_h(K, PB, identf, modT, X1, gi, 0, xt[s % 2], hT32, hT, s)
            pr = PB[2]
            hs_ = slice(128 * s, 128 * s + 128)
            tt(K, "dve", hlo[:], hT32[:], hT[:, :, hs_], ALU.subtract, rd=[hT32, hT], wr=[hlo])
            for k in range(8):
                mm(K, pr[:, 0:NE], hT[:, k, hs_], rwh[:, k, :], rd=[hT, rwh], wr=[pr], start=k == 0, stop=False)
                mm(K, pr[:, 0:NE], hlo[:, k, :], rwh[:, k, :], rd=[hlo, rwh], wr=[pr], start=False, stop=False)
                mm(K, pr[:, 0:NE], hT[:, k, hs_], rwl[:, k, :], rd=[hT, rwl], wr=[pr], start=False, stop=k == 7)
            tt(K, "dve", lg[:, 0:8], pr[:, 0:NE], rbB[:], ALU.add, rd=[pr, rbB], wr=[lg])
            K.op("dve", lambda e: e.max(out=lg[:, 8:16], in_=lg[:, 0:8]), rd=[lg], wr=[lg])
            ts(K, "dve", lg[:, 16:24], lg[:, 0:8], lg[:, 8:9], None, ALU.subtract, None, rd=[lg], wr=[lg])
            act(K, lg[:, 16:24], lg[:, 16:24], AF.Exp, rd=[lg], wr=[lg])
            ts(K, "dve", lg[:, 24:32], lg[:, 0:8], lg[:, 9:10], None, ALU.is_ge, None, rd=[lg], wr=[lg])
            tt(K, "dve", lg[:, 16:24], lg[:, 16:24], lg[:, 24:32], ALU.mult, rd=[lg], wr=[lg])
            K.op("dve", lambda e: e.tensor_reduce(out=lg[:, 24:25], in_=lg[:, 16:24], axis=AX.X, op=ALU.add), rd=[lg], wr=[lg])
            K.op("dve", lambda e: e.reciprocal(out=lg[:, 25:26], in_=lg[:, 24:25]), rd=[lg], wr=[lg])
            ts(K, "dve", comb[:, s, :], lg[:, 16:24], lg[:, 25:26], None, ALU.mult, None, rd=[lg], wr=[comb])
        for ex in range(NE):
            guv = moe_gu[0, ex].rearrange("(k p) n -> p k n", p=128)
            dnv = moe_dn[0, ex].rearrange("(c p) n -> p c n", p=128)
            wd = wdn[ex % 2]
            K.dma("pool", wd[:], dnv, wr=wd)
            for c in range(NCH):
                wb = wgu[cntw % 3]
                cntw += 1
                K.dma("pool", wb[:, :, 0, :], guv[:, :, 128 * c:128 * c + 128], wr=wb)
                K.dma("pool", wb[:, :, 1, :], guv[:, :, D_EXP + 128 * c:D_EXP + 128 * c + 128], wr=wb)
                for hf in range(NH):
                    ts_ = slice(512 * hf, 512 * hf + 512)
                    pg, pu = PB[2 + (hf % 2) * 2], PB[3 + (hf % 2) * 2]
                    for k in range(8):
                        mm(K, pg[:, :], wb[:, k, 0, :], hT[:, k, ts_], rd=[wb, hT], wr=[pg], start=k == 0, stop=k == 7)
                    for k in range(8):
                        mm(K, pu[:, :], wb[:, k, 1, :], hT[:, k, ts_], rd=[wb, hT], wr=[pu], start=k == 0, stop=k == 7)
                    sgt = sg[hf % 2]
                    act(K, sgt[:], pg[:, :], AF.Silu, rd=[pg], wr=[sgt])
                    tt(K, "dve", hid[:, c, ts_], sgt[:], pu[:, :], ALU.mult, rd=[sgt, pu], wr=[hid])
            for s in range(MT):
                for hf in range(2):
                    pz = PB[hf]
                    cs = slice(512 * hf, 512 * hf + 512)
                    for c in range(NCH):
                        mm(K, pz[:, :], hid[:, c, 128 * s:128 * s + 128], wd[:, c, cs], rd=[hid, wd], wr=[pz],
                           start=c == 0, stop=c == NCH - 1)
                    if ex == 0:
                        ts(K, "dve", acc[:, s, cs], pz[:, :], comb[:, s, ex:ex + 1], None, ALU.mult, None, rd=[pz, comb], wr=[acc])
                    else:
                        stt(K, acc[:, s, cs], pz[:, :], comb[:, s, ex:ex + 1], acc[:, s, cs], ALU.mult, ALU.add,
                            rd=[pz, comb, acc], wr=[acc])
        for s, gi in enumerate(tiles):
            tt(K, "dve", tmpA[:], acc[:, s, :], G[(5, 0)][:], ALU.mult, rd=[acc, G[(5, 0)]], wr=[tmpA])
            xr = xt[s % 2]
            K.dma("sp", xr[:], X1[128 * gi:128 * gi + 128, :], wr=xr)
            stt(K, tmpA[:], xr[:], ALPHA, tmpA[:], ALU.mult, ALU.add, rd=[xr, tmpA], wr=[tmpA])
            layer_norm(K, tmpA, xo, lng, lnb, lnscr)
            K.dma("sp", out_d[128 * (gi - NCT):128 * (gi - NCT) + 128, :], xo[:], rd=xo)


_CACHE = {}


def _core_inputs(inputs, b):
    f = lambda a: np.ascontiguousarray(np.asarray(a, dtype=np.float32))
    m = {
        "x": f(inputs["x"][b]),
        "c": f(inputs["c"][b:b + 1]),
        "ctx": f(inputs["ctx"][b]),
        "c_ctx": f(np.asarray(inputs["c_ctx"]).reshape(1, D)),
        "ml_gate_bias": f(np.asarray(inputs["ml_gate_bias"]).reshape(DEPTH, 16)),
        "hg_norm": f(np.asarray(inputs["hg_norm"]).reshape(DEPTH, 512)),
        "ml_norm": f(np.asarray(inputs["ml_norm"]).reshape(DEPTH, 512)),
    }
    for k in ("w_ada", "b_ada", "w_in", "ml_conv_w", "ml_conv_b", "hg_lower_bound", "w_out", "ln_g", "ln_b",
              "ffn_w_gate_up", "ffn_w_down", "router_w", "router_b", "moe_w_gate_up", "moe_w_down"):
        m[k] = f(inputs[k])
    return m


def kernel(**inputs):
    x = np.asarray(inputs["x"])
    B, SEQ, _ = x.shape
    if SEQ not in _CACHE:
        _CACHE[SEQ] = build_program(SEQ)
    nc = _CACHE[SEQ]
    in_maps = [_core_inputs(inputs, b) for b in range(B)]
    res = run_bass_kernel_spmd(nc, in_maps, core_ids=list(range(B)))
    return np.stack([np.asarray(r["out"], dtype=np.float32) for r in res.results], axis=0)
```
